# Optimizing a Trainium2 kernel written in Bass

```python
import jax
import jax.numpy as jnp
from jax import lax
import numpy as np

D_MODEL = 1024
BATCH = 16
SEQ = 2048
DEPTH = 4

MIX_WIDTH = D_MODEL
POOL_WIDTH = MIX_WIDTH // 2
POOL_WINDOWS = (2, 4, 8, 16)
N_POOL_GROUPS = len(POOL_WINDOWS)
POOL_CH = POOL_WIDTH // N_POOL_GROUPS
MAX_WINDOW = max(POOL_WINDOWS)
SB_WIDTH = MIX_WIDTH - POOL_WIDTH
SB_HEAD_DIM = 64
SB_HEADS = SB_WIDTH // SB_HEAD_DIM
Q_BLOCK = 128
PROJ_WIDTH = POOL_WIDTH + 3 * SB_WIDTH
N_EXPERT_GROUPS = 4
EXPERTS_PER_GROUP = 8
N_EXPERTS = N_EXPERT_GROUPS * EXPERTS_PER_GROUP
TOP_K = 2
D_EXPERT = D_MODEL // 2
DISPATCH_BLOCK = 128
DEEPNORM_ALPHA = (2.0 * DEPTH) ** 0.25
DEEPNORM_BETA = (8.0 * DEPTH) ** -0.25
LN_EPS = 1e-5

kernel_name = "hybrid_pool_stickbreak_hmoe"


def layer_norm(x, g, b):
    xf = x.astype(jnp.float32)
    mu = jnp.mean(xf, axis=-1, keepdims=True)
    xc = xf - mu
    var = jnp.mean(xc * xc, axis=-1, keepdims=True)
    y = xc * lax.rsqrt(var + LN_EPS) * g.astype(jnp.float32) + b.astype(jnp.float32)
    return y.astype(x.dtype)


def multiscale_pool(u, w_pool, pool_scale):
    B, S, _ = u.shape
    ug = u.reshape(B, S, N_POOL_GROUPS, POOL_CH).astype(jnp.float32)
    csum = jnp.cumsum(ug, axis=1)
    cpad = jnp.concatenate(
        [jnp.zeros((B, MAX_WINDOW, N_POOL_GROUPS, POOL_CH), jnp.float32), csum], axis=1)
    pos = jnp.arange(S, dtype=jnp.float32)
    sums, counts = [], []
    for g, w in enumerate(POOL_WINDOWS):
        lo = cpad[:, MAX_WINDOW - w:MAX_WINDOW - w + S, g]
        sums.append(csum[:, :, g] - lo)
        counts.append(jnp.minimum(pos + 1.0, float(w)))
    wsum = jnp.stack(sums, axis=2)
    count = jnp.stack(counts, axis=1)
    pooled = wsum / count[None, :, :, None] - ug
    mixed = jnp.einsum('bsgc,gcd->bsgd', pooled.astype(u.dtype), w_pool)
    return mixed.reshape(B, S, POOL_WIDTH) * pool_scale


def stick_breaking_attention(q, k, v):
    B, S, H, Dh = q.shape
    n_blocks = S // Q_BLOCK
    scale = Dh ** -0.5
    qb = q.reshape(B, n_blocks, Q_BLOCK, H, Dh).transpose(1, 0, 3, 2, 4)
    kh = k.transpose(0, 2, 1, 3)
    vh = v.transpose(0, 2, 1, 3)
    key_pos = jnp.arange(S)

    def block(args):
        qi, bi = args
        z = jnp.einsum('bhqd,bhkd->bhqk', qi, kh).astype(jnp.float32) * scale
        q_pos = bi * Q_BLOCK + jnp.arange(Q_BLOCK)
        mask = key_pos[None, :] < q_pos[:, None]
        log_fail = jnp.where(mask, jax.nn.log_sigmoid(-z), 0.0)
        after = lax.cumsum(log_fail, axis=3, reverse=True) - log_fail
        a = jnp.where(mask, jnp.exp(jax.nn.log_sigmoid(z) + after), 0.0)
        return jnp.einsum('bhqk,bhkd->bhqd', a.astype(vh.dtype), vh)

    o = lax.map(block, (qb, jnp.arange(n_blocks)))
    return o.transpose(1, 0, 3, 2, 4).reshape(B, S, H * Dh)


def hierarchical_moe(h, w_rg, b_rg, w_re, b_re, w_gate, w_up, w_down):
    B, S, D = h.shape
    T = B * S
    xt = h.reshape(T, D)
    tok_idx = jnp.arange(T)
    g_logits = (xt @ w_rg).astype(jnp.float32) + b_rg.astype(jnp.float32)
    g_prob = jax.nn.softmax(g_logits, axis=-1)
    g_idx = jnp.argmax(g_logits, axis=-1).astype(jnp.int32)
    g_p = g_prob[tok_idx, g_idx]
    e_logits = (xt @ w_re).astype(jnp.float32).reshape(T, N_EXPERT_GROUPS, EXPERTS_PER_GROUP)
    e_logits = e_logits + b_re.astype(jnp.float32)
    e_prob = jax.nn.softmax(e_logits[tok_idx, g_idx], axis=-1)
    top_p, top_i = lax.top_k(e_prob, TOP_K)
    top_p = top_p / jnp.sum(top_p, axis=-1, keepdims=True)
    gate = g_p[:, None] * top_p
    expert = g_idx[:, None] * EXPERTS_PER_GROUP + top_i.astype(jnp.int32)

    A = T * TOP_K
    flat_e = expert.reshape(A)
    flat_w = gate.reshape(A)
    flat_tok = jnp.arange(A, dtype=jnp.int32) // TOP_K
    order = jnp.argsort(flat_e)
    se, stok, sw = flat_e[order], flat_tok[order], flat_w[order]
    counts = jax.ops.segment_sum(jnp.ones((A,), jnp.int32), flat_e, num_segments=N_EXPERTS)
    start = jnp.cumsum(counts) - counts
    padded = ((counts + DISPATCH_BLOCK - 1) // DISPATCH_BLOCK) * DISPATCH_BLOCK
    pstart = jnp.cumsum(padded) - padded
    pend = pstart + padded
    dest = pstart[se] + (jnp.arange(A, dtype=jnp.int32) - start[se])
    n_blocks = -(-A // DISPATCH_BLOCK) + N_EXPERTS
    P = n_blocks * DISPATCH_BLOCK
    slot_tok = jnp.zeros((P,), jnp.int32).at[dest].set(stok)
    slot_w = jnp.zeros((P,), jnp.float32).at[dest].set(sw)
    block_start = jnp.arange(n_blocks, dtype=jnp.int32) * DISPATCH_BLOCK
    block_e = jnp.minimum(jnp.sum(block_start[:, None] >= pend[None, :], axis=1),
                          N_EXPERTS - 1).astype(jnp.int32)
    xs = xt[slot_tok].reshape(n_blocks, DISPATCH_BLOCK, D)

    def expert_block(args):
        xb, e = args
        hid = jax.nn.silu(xb @ w_gate[e]) * (xb @ w_up[e])
        return hid @ w_down[e]

    ys = lax.map(expert_block, (xs, block_e)).reshape(P, D)
    out = jnp.zeros((T, D), ys.dtype).at[slot_tok].add(ys * slot_w[:, None].astype(ys.dtype))
    return out.reshape(B, S, D).astype(h.dtype)


def setup_inputs(seed: int = 0) -> dict:
    key = jax.random.key(seed)
    ks = jax.random.split(key, 17)
    nrm = jax.random.normal
    f32 = jnp.float32
    col_scale = jnp.concatenate([
        jnp.full((POOL_WIDTH + 2 * SB_WIDTH,), D_MODEL ** -0.5, f32),
        jnp.full((SB_WIDTH,), D_MODEL ** -0.5 * DEEPNORM_BETA, f32)])
    return {
        "x": nrm(ks[0], (BATCH, SEQ, D_MODEL), f32),
        "w_in": nrm(ks[1], (DEPTH, D_MODEL, PROJ_WIDTH), f32) * col_scale,
        "w_pool": nrm(ks[2], (DEPTH, N_POOL_GROUPS, POOL_CH, POOL_CH), f32) * POOL_CH ** -0.5,
        "pool_scale": 1.0 + 0.1 * nrm(ks[3], (DEPTH, POOL_WIDTH), f32),
        "w_out": nrm(ks[4], (DEPTH, MIX_WIDTH, D_MODEL), f32) * (MIX_WIDTH ** -0.5 * DEEPNORM_BETA),
        "ln1_g": 1.0 + 0.01 * nrm(ks[5], (DEPTH, D_MODEL), f32),
        "ln1_b": 0.01 * nrm(ks[6], (DEPTH, D_MODEL), f32),
        "w_router_group": nrm(ks[7], (DEPTH, D_MODEL, N_EXPERT_GROUPS), f32) * D_MODEL ** -0.5,
        "b_router_group": 0.01 * nrm(ks[8], (DEPTH, N_EXPERT_GROUPS), f32),
        "w_router_expert": nrm(ks[9], (DEPTH, D_MODEL, N_EXPERTS), f32) * D_MODEL ** -0.5,
        "b_router_expert": 0.01 * nrm(ks[10], (DEPTH, N_EXPERT_GROUPS, EXPERTS_PER_GROUP), f32),
        "w_gate": nrm(ks[11], (DEPTH, N_EXPERTS, D_MODEL, D_EXPERT), f32) * D_MODEL ** -0.5,
        "w_up": nrm(ks[12], (DEPTH, N_EXPERTS, D_MODEL, D_EXPERT), f32) * D_MODEL ** -0.5,
        "w_down": nrm(ks[13], (DEPTH, N_EXPERTS, D_EXPERT, D_MODEL), f32) * (D_EXPERT ** -0.5 * DEEPNORM_BETA),
        "ln2_g": 1.0 + 0.01 * nrm(ks[14], (DEPTH, D_MODEL), f32),
        "ln2_b": 0.01 * nrm(ks[15], (DEPTH, D_MODEL), f32),
    }


def reference(x, w_in, w_pool, pool_scale, w_out, ln1_g, ln1_b, w_router_group, b_router_group,
              w_router_expert, b_router_expert, w_gate, w_up, w_down, ln2_g, ln2_b):
    B, S, _ = x.shape
    h = x
    for l in range(DEPTH):
        proj = h @ w_in[l]
        u_pool = proj[..., :POOL_WIDTH]
        q = proj[..., POOL_WIDTH:POOL_WIDTH + SB_WIDTH].reshape(B, S, SB_HEADS, SB_HEAD_DIM)
        k = proj[..., POOL_WIDTH + SB_WIDTH:POOL_WIDTH + 2 * SB_WIDTH].reshape(B, S, SB_HEADS, SB_HEAD_DIM)
        v = proj[..., POOL_WIDTH + 2 * SB_WIDTH:].reshape(B, S, SB_HEADS, SB_HEAD_DIM)
        y_pool = multiscale_pool(u_pool, w_pool[l], pool_scale[l])
        y_sb = stick_breaking_attention(q, k, v)
        mix = jnp.concatenate([y_pool.astype(h.dtype), y_sb.astype(h.dtype)], axis=-1) @ w_out[l]
        h = layer_norm(DEEPNORM_ALPHA * h + mix, ln1_g[l], ln1_b[l])
        moe = hierarchical_moe(h, w_router_group[l], b_router_group[l], w_router_expert[l],
                               b_router_expert[l], w_gate[l], w_up[l], w_down[l])
        h = layer_norm(DEEPNORM_ALPHA * h + moe, ln2_g[l], ln2_b[l])
    return h
```

```python
import numpy as np
import ml_dtypes
import concourse.bass as bass
import concourse.mybir as mybir
from concourse.bass_utils import run_bass_kernel_spmd

F32 = mybir.dt.float32
BF16 = mybir.dt.bfloat16
I32 = mybir.dt.int32
AF = mybir.ActivationFunctionType
ALU = mybir.AluOpType
AX = mybir.AxisListType

D = 1024
DC = 8
NH = 8
HD = 64
PW = 512
NE = 32
NG = 4
EPG = 8
DE = 512
WINS = (2, 4, 8, 16)
LN_EPS = 1e-5
ALPHA = (2.0 * 4) ** 0.25
BIG = 1.0e30


class Buf:
    __slots__ = ("name", "w", "r")

    def __init__(self, name):
        self.name = name
        self.w = {}
        self.r = {}


class Eng:
    def __init__(self, fw, name, h, compute=True):
        self.fw = fw
        self.name = name
        self.h = h
        self.compute = compute
        self.seen = {}
        self.count = 0
        self.sem = None
        self.key = None
        self.pool = []
        self.pool_i = 0


class FW:
    def __init__(self, nc, n_dma_sems=10):
        self.nc = nc
        self.epoch = 0
        self.bufs = []
        self.pe = Eng(self, "pe", nc.tensor)
        self.act = Eng(self, "act", nc.scalar)
        self.dve = Eng(self, "dve", nc.vector)
        self.pool = Eng(self, "pool", nc.gpsimd)
        self.sp = Eng(self, "sp", nc.sync, compute=False)
        self.engs = [self.pe, self.act, self.dve, self.pool, self.sp]
        for e in self.engs:
            if e.compute:
                self._new_sem(e)
        for e in (self.sp, self.pool, self.act):
            for i in range(n_dma_sems):
                s = nc.alloc_semaphore(f"dq_{e.name}_{i}")
                e.pool.append([f"dq_{e.name}_{i}", s, 0])
        self.n_inst = 0
        self.dead = False
        _FW[0] = self

    def _new_sem(self, e):
        e.sem = self.nc.alloc_semaphore(f"s_{e.name}_{self.epoch}")
        e.key = f"{e.name}@{self.epoch}"
        e.count = 0

    def buf(self, name):
        b = Buf(name)
        self.bufs.append(b)
        return b

    def _wait(self, e, key, sem, val):
        if e.seen.get(key, 0) >= val:
            return
        e.h.wait_ge(sem, val)
        e.seen[key] = val

    def _deps(self, e, reads, writes, dma, cwrites=()):
        for b in reads:
            for k, (s, v) in b.w.items():
                self._wait(e, k, s, v)
        for b in cwrites:
            for k, (s, v) in b.r.items():
                if dma or k != e.key:
                    self._wait(e, k, s, v)
        for b in writes:
            for k, (s, v) in b.w.items():
                if dma or k != e.key:
                    self._wait(e, k, s, v)
            for k, (s, v) in b.r.items():
                if dma or k != e.key:
                    self._wait(e, k, s, v)

    def _mark(self, key, sem, val, reads, writes, cwrites=()):
        for b in reads:
            b.r[key] = (sem, val)
        for b in cwrites:
            b.w[key] = (sem, val)
        for b in writes:
            b.w = {key: (sem, val)}
            b.r = {}

    def op(self, e, fn, reads=(), writes=(), inc=True, cwrites=()):
        if self.dead:
            return None
        self._deps(e, reads, writes, False, cwrites)
        ins = fn()
        self.n_inst += 1
        if inc:
            e.count += 1
            ins.then_inc(e.sem, 1)
            val = e.count
        else:
            val = e.count + 1
        self._mark(e.key, e.sem, val, reads, writes, cwrites)
        return ins

    def dma(self, e, fn, reads=(), writes=(), cwrites=()):
        if self.dead:
            return None
        slot = e.pool[e.pool_i]
        e.pool_i = (e.pool_i + 1) % len(e.pool)
        key, sem, val = slot
        if val:
            self._wait(e, key, sem, val)
        self._deps(e, reads, writes, True, cwrites)
        ins = fn()
        self.n_inst += 1
        val += 16
        slot[2] = val
        ins.then_inc(sem, 16)
        self._mark(key, sem, val, reads, writes, cwrites)
        return (key, sem, val)

    def barrier(self):
        if self.dead:
            return
        comp = [e for e in self.engs if e.compute]
        for e in self.engs:
            for x in comp:
                if x.count:
                    self._wait(e, x.key, x.sem, x.count)
            for q in (self.sp, self.pool, self.act):
                for key, sem, val in q.pool:
                    if val:
                        self._wait(e, key, sem, val)
        for b in self.bufs:
            b.w = {}
            b.r = {}
        if max(e.count for e in comp) > 12000:
            self.epoch += 1
            for e in comp:
                self._new_sem(e)


class Rot:
    def __init__(self, fw, es, name, shape, dtype, n, psum=False):
        self.items = []
        for i in range(n):
            _UID[0] += 1
            if psum:
                t = es.enter_context(fw.nc.psum_tensor(f"{name}{i}_{_UID[0]}", shape, dtype))
            else:
                t = es.enter_context(fw.nc.sbuf_tensor(f"{name}{i}_{_UID[0]}", shape, dtype))
            self.items.append((t, fw.buf(f"{name}{i}")))
        self.i = 0

    def next(self):
        it = self.items[self.i]
        self.i = (self.i + 1) % len(self.items)
        return it


class _Stop(Exception):
    pass


_FW = [None]
_UID = [0]


def _chk(tag):
    import os
    if os.environ.get("MK_STOP") == tag and not _FW[0].dead:
        _FW[0].barrier()
        _FW[0].dead = True


class Cfg:
    def __init__(self, S=2048, NSEQ=2, L=4, C=384, stop_after=None):
        self.S, self.NSEQ, self.L, self.C = S, NSEQ, L, C
        self.T = S * NSEQ
        self.NB = self.T // 128
        self.SB = S // 128
        self.NCH = S // 512
        self.P = NE * C
        self.CB = C // 128
        self.stop_after = stop_after


def make_consts(cfg):
    c = np.zeros((128, 640), np.float32)
    c[:, 608] = LN_EPS
    c[:, 609] = 1.0
    r = np.arange(128)
    c[:, 0:128] = np.eye(128)
    c[:, 128:256] = (r[:, None] >= r[None, :])
    c[:, 256:384] = 1.0
    c[:, 384:512] = (r[:, None] < r[None, :])
    c[:, 512:544] = (np.arange(NE) * cfg.C)[None, :]
    for g, w in enumerate(WINS):
        c[:, 544 + g * 16: 544 + (g + 1) * 16] = 1.0 / np.minimum(np.arange(16) + 1.0, float(w))[None, :]
    return c


def build(cfg):
    from contextlib import ExitStack
    nc = bass.Bass("TRN2", target_bir_lowering=False)
    fw = FW(nc)
    pe, act, dve, pool, sp = fw.pe, fw.act, fw.dve, fw.pool, fw.sp
    S, NSEQ, L, C, T, NB, SB, NCH, P, CB = (cfg.S, cfg.NSEQ, cfg.L, cfg.C, cfg.T, cfg.NB, cfg.SB,
                                            cfg.NCH, cfg.P, cfg.CB)
    PSW = 1024

    def din(name, shape):
        return nc.dram_tensor(name, list(shape), F32, kind="ExternalInput").ap()

    x = din("x", [T, D])
    w_in = din("w_in", [L, D, 2048])
    w_pool = din("w_pool", [L, 4, 128, 128])
    pool_scale = din("pool_scale", [L, 512])
    w_out = din("w_out", [L, D, D])
    ln1_g = din("ln1_g", [L, D]); ln1_b = din("ln1_b", [L, D])
    w_rg = din("w_rg", [L, D, NG]); b_rg = din("b_rg", [L, NG])
    w_re = din("w_re", [L, D, NE]); b_re = din("b_re", [L, NE])
    w_gate = din("w_gate", [L, NE, D, DE]); w_up = din("w_up", [L, NE, D, DE])
    w_down = din("w_down", [L, NE, DE, D])
    ln2_g = din("ln2_g", [L, D]); ln2_b = din("ln2_b", [L, D])
    cst = din("cst", [128, 640])
    out = nc.dram_tensor("out", [T, D], F32, kind="ExternalOutput").ap()

    def dscr(name, shape, dt):
        return nc.dram_tensor(name, list(shape), dt, kind="Internal").ap()

    hA = dscr("hA", [T, D], F32)
    h1d = dscr("h1d", [T, D], F32)
    h1bd = dscr("h1bd", [T, D], BF16)
    xsd = dscr("xsd", [P + 128, D], BF16)
    ysd = dscr("ysd", [P + 128, D], F32)
    wbg = dscr("wbg", [NE, D, DE], BF16)
    wbu = dscr("wbu", [NE, D, DE], BF16)
    wbd = dscr("wbd", [NE, DE, D], BF16)
    dbg = None
    if cfg.stop_after is not None:
        dbg = True

    hA_b = [fw.buf(f"hA{j}") for j in range(NB)]
    h1d_b = [fw.buf(f"h1d{j}") for j in range(NB)]
    h1bd_b = [fw.buf(f"h1bd{j}") for j in range(NB)]
    xsd_b = fw.buf("xsd")
    ysd_b = fw.buf("ysd")
    out_b = [fw.buf(f"out{j}") for j in range(NB)]
    win_b = fw.buf("weights")
    wb_b = [fw.buf(f"wb{e}") for e in range(NE)]
    pending = []

    es = ExitStack()
    with es:
        def sb_t(stack, name, shape, dt):
            _UID[0] += 1
            return stack.enter_context(nc.sbuf_tensor(f"{name}_{_UID[0]}", list(shape), dt))

        def ps_t(stack, name, shape, dt):
            _UID[0] += 1
            return stack.enter_context(nc.psum_tensor(f"{name}_{_UID[0]}", list(shape), dt))

        reg_sc = nc.gpsimd.to_reg(P - 1)
        reg_ga = nc.gpsimd.to_reg(P + 127)
        cf = sb_t(es, "cf", [128, 640], F32); cf_b = fw.buf("cf")
        cb = sb_t(es, "cb", [128, 512], BF16); cb_b = fw.buf("cb")
        fw.dma(sp, lambda: nc.sync.dma_start(out=cf[:], in_=cst[:, :]), reads=[win_b], writes=[cf_b])
        fw.op(dve, lambda: nc.vector.tensor_copy(out=cb[:], in_=cf[:, 0:512]), reads=[cf_b], writes=[cb_b])
        ident = cb[:, 0:128]
        triU = cb[:, 128:256]
        ones = cb[:, 256:384]
        triL = cb[:, 384:512]
        maskf = cf[:, 384:512]
        maskb = cb[:, 384:512]
        eC = cf[:, 512:544]
        epsc = cf[:, 608:609]
        onec = cf[:, 609:610]
        L_t = sb_t(es, "L_t", [128, NB, 36], F32); L_b = fw.buf("L")
        d1i = sb_t(es, "d1i", [128, NB], I32); d2i = sb_t(es, "d2i", [128, NB], I32)
        g1 = sb_t(es, "g1", [128, NB], F32); g2 = sb_t(es, "g2", [128, NB], F32)
        rt_b = fw.buf("route")
        with ExitStack() as zs:
            zf = sb_t(zs, "zf", [128, D], F32); zf_b = fw.buf("zf")
            zb = sb_t(zs, "zb", [128, D], BF16); zb_b = fw.buf("zb")
            fw.op(pool, lambda: nc.gpsimd.memset(zf[:], 0.0), writes=[zf_b])
            fw.op(pool, lambda: nc.gpsimd.memset(zb[:], 0.0), writes=[zb_b])
            fw.dma(sp, lambda: nc.sync.dma_start(out=ysd[P:P + 128, :], in_=zf[:]), reads=[zf_b], cwrites=[ysd_b])
            nrow = (P + 128) // 128
            r0 = 0
            import os
            if os.environ.get("MK_SKIP_ZERO"):
                r0 = nrow
            while r0 < nrow:
                rn = min(16, nrow - r0)
                dst = xsd[r0 * 128:(r0 + rn) * 128, :].rearrange("(r p) d -> p r d", p=128)
                srcz = zb[:, :].unsqueeze(1).broadcast_to([128, rn, D])
                fw.dma(sp, lambda: nc.sync.dma_start(out=dst, in_=srcz), reads=[zb_b], cwrites=[xsd_b])
                r0 += rn
            fw.barrier()

        def transpose_block(src_t, src_b, tp_rot, dst_ap, dst_b, copy_eng):
            t_tp, b_tp = tp_rot.next()
            for dc in range(DC):
                fw.op(pe, lambda: nc.tensor.transpose(out=t_tp[:, dc, :], in_=src_t[:, dc * 128:(dc + 1) * 128],
                                                      identity=ident),
                      reads=[src_b, cb_b], writes=[b_tp], inc=(dc == DC - 1))
            if copy_eng is act:
                fw.op(act, lambda: nc.scalar.copy(out=dst_ap, in_=t_tp[:]), reads=[b_tp], writes=[dst_b])
            else:
                fw.op(dve, lambda: nc.vector.tensor_copy(out=dst_ap, in_=t_tp[:]), reads=[b_tp], writes=[dst_b])

        def ln_stats(z_t, z_b, st_rot):
            st_t, st_b = st_rot.next()
            for i in range(2):
                fw.op(dve, lambda: nc.vector.bn_stats(out=st_t[:, i * 6:(i + 1) * 6], in_=z_t[:, i * 512:(i + 1) * 512]),
                      reads=[z_b], writes=[st_b])
            fw.op(dve, lambda: nc.vector.bn_aggr(out=st_t[:, 12:14], in_=st_t[:, 0:12]), reads=[st_b], writes=[st_b])
            fw.op(dve, lambda: nc.vector.tensor_scalar(out=st_t[:, 11:12], in0=st_t[:, 12:13], scalar1=-1.0, scalar2=None, op0=ALU.mult),
                  reads=[st_b], writes=[st_b])
            fw.op(act, lambda: nc.scalar.activation(out=st_t[:, 14:15], in_=st_t[:, 13:14], func=AF.Ln, bias=epsc[:, 0:1]),
                  reads=[st_b, cf_b], writes=[st_b])
            fw.op(act, lambda: nc.scalar.activation(out=st_t[:, 14:15], in_=st_t[:, 14:15], func=AF.Exp, scale=-0.5),
                  reads=[st_b], writes=[st_b])
            fw.op(act, lambda: nc.scalar.activation(out=st_t[:, 15:16], in_=st_t[:, 11:12], func=AF.Copy, scale=st_t[:, 14:15]),
                  reads=[st_b], writes=[st_b])
            return st_t, st_b

        def ln_apply(z_t, z_b, st_t, st_b, gB, bB, gb_b, zn_rot, h_rot, badd_eng=None):
            zn_t, zn_b = zn_rot.next()
            fw.op(act, lambda: nc.scalar.activation(out=zn_t[:], in_=z_t[:], func=AF.Identity,
                                                    bias=st_t[:, 15:16], scale=st_t[:, 14:15]),
                  reads=[z_b, st_b], writes=[zn_b])
            h_t, h_b = h_rot.next()
            fw.op(dve, lambda: nc.vector.tensor_tensor(out=h_t[:], in0=zn_t[:], in1=gB[:], op=ALU.mult),
                  reads=[zn_b, gb_b], writes=[h_b])
            if badd_eng is dve:
                fw.op(dve, lambda: nc.vector.tensor_tensor(out=h_t[:], in0=h_t[:], in1=bB[:], op=ALU.add),
                      reads=[h_b, gb_b], writes=[h_b])
            else:
                fw.op(pool, lambda: nc.gpsimd.tensor_tensor(out=h_t[:], in0=h_t[:], in1=bB[:], op=ALU.add),
                      reads=[h_b, gb_b], writes=[h_b])
            return h_t, h_b

        def layer_norm(z_t, z_b, gB, bB, gb_b, st_rot, zn_rot, h_rot):
            st_t, st_b = ln_stats(z_t, z_b, st_rot)
            return ln_apply(z_t, z_b, st_t, st_b, gB, bB, gb_b, zn_rot, h_rot)

        def pipeline(iters):
            n = len(iters)
            ns = max(len(i) for i in iters) if iters else 0
            for step in range(n + ns - 1):
                for s in range(ns - 1, -1, -1):
                    i = step - s
                    if 0 <= i < n and s < len(iters[i]):
                        iters[i][s]()

        try:
          for l in range(L):
              h_src = x if l == 0 else hA
              hsrc_b = [win_b] * NB if l == 0 else hA_b
              last = (l == L - 1)

              def mkcast(e, dst, srcw):
                  def f():
                      fw.dma(pool, lambda: nc.gpsimd.dma_start(out=dst[e], in_=srcw[l, e]), reads=[win_b], cwrites=[wb_b[e]])
                  return f
              for e in range(NE):
                  pending.append(mkcast(e, wbg, w_gate))
                  pending.append(mkcast(e, wbu, w_up))
                  pending.append(mkcast(e, wbd, w_down))
              n_att_it = [0]
              tot_it = NSEQ * 4 * 2 * sum(4 * qc + 4 for qc in range(NCH))
              cast_every = max(1, (tot_it * 9 // 10) // (3 * NE))
              for b in range(NSEQ):
                  if cfg.stop_after == ("Z", l):
                      continue
                  seq = ExitStack()
                  with seq:
                      ycatT = sb_t(seq, "ycatT", [128, 8, S], BF16); ycat_b = fw.buf("ycatT")
                      v_t = sb_t(seq, "v_t", [128, SB, 512], BF16); v_b = fw.buf("v")
                      qk = sb_t(seq, "qk", [128, 2, 4, S], BF16)
                      wo = sb_t(seq, "wo", [128, 8, D], BF16); wo_b = fw.buf("wo")
                      q_b = fw.buf("q"); k_b = fw.buf("k"); kn_b = fw.buf("kn")
                      ph = ExitStack()
                      with ph:
                          hT = sb_t(ph, "hT", [128, DC, S], BF16); hT_b = fw.buf("hT")
                          W = sb_t(ph, "W", [128, DC, 1024], BF16); W_b = fw.buf("W")
                          wpl = sb_t(ph, "wpl", [128, 4, 128], BF16); wpl_b = fw.buf("wpl")
                          psc = sb_t(ph, "psc", [128, 4], F32); psc_b = fw.buf("psc")
                          xin = Rot(fw, ph, "xin", [128, D], F32, 2)
                          xbf = Rot(fw, ph, "xbf", [128, D], BF16, 2)
                          tp = Rot(fw, ph, "tpP", [128, DC, 128], BF16, 2, psum=True)
                          mm = Rot(fw, ph, "mmP", [128, 512], F32, 4, psum=True)
                          u_t = sb_t(ph, "u_t", [128, 16 + S], F32); u_b = fw.buf("u")
                          sa_t = sb_t(ph, "sa_t", [128, 16 + S], F32); sa_b = fw.buf("sa")
                          sb2_t = sb_t(ph, "sb2_t", [128, 16 + S], F32); sb2_b = fw.buf("sb2")
                          pl_t = sb_t(ph, "pl_t", [128, S], BF16); pl_b = fw.buf("pl")
                          f16 = sb_t(ph, "f16", [128, 16], F32); f16_b = fw.buf("f16")
                          win_l = w_in[l].rearrange("(c p) n -> p c n", p=128)
                          fw.dma(pool, lambda: nc.gpsimd.dma_start(out=W[:, :, 0:512], in_=win_l[:, :, 0:512]),
                                 reads=[win_b], writes=[W_b])
                          fw.dma(pool, lambda: nc.gpsimd.dma_start(out=W[:, :, 512:1024], in_=win_l[:, :, 1536:2048]),
                                 reads=[win_b], cwrites=[W_b])
                          fw.dma(pool, lambda: nc.gpsimd.dma_start(out=wpl[:], in_=w_pool[l].rearrange("g c d -> c g d")),
                                 reads=[win_b], writes=[wpl_b])
                          with nc.allow_non_contiguous_dma(reason="tiny pool scale"):
                              fw.dma(sp, lambda: nc.sync.dma_start(out=psc[:], in_=pool_scale[l].rearrange("(g p) -> p g", p=128)),
                                     reads=[win_b], writes=[psc_b])
                          for t_, b_ in ((u_t, u_b), (sa_t, sa_b), (sb2_t, sb2_b)):
                              fw.op(pool, lambda: nc.gpsimd.memset(t_[:, 0:16], 0.0), writes=[b_])
                          _chk("P1")
                          for jb in range(SB):
                              j = b * SB + jb
                              t_in, b_in = xin.next()
                              fw.dma(sp, lambda: nc.sync.dma_start(out=t_in[:], in_=h_src[j * 128:(j + 1) * 128, :]),
                                     reads=[hsrc_b[j]], writes=[b_in])
                              t_bf, b_bf = xbf.next()
                              fw.op(dve, lambda: nc.vector.tensor_copy(out=t_bf[:], in_=t_in[:]), reads=[b_in], writes=[b_bf])
                              transpose_block(t_bf, b_bf, tp, hT[:, :, jb * 128:(jb + 1) * 128], hT_b, act)
                          _chk("P2")
                          def emit_v(jbs):
                              for jb in jbs:
                                  t_mm, b_mm = mm.next()
                                  for dc in range(DC):
                                      fw.op(pe, lambda: nc.tensor.matmul(t_mm[:, :], lhsT=hT[:, dc, jb * 128:(jb + 1) * 128],
                                                                         rhs=W[:, dc, 512:1024], start=(dc == 0), stop=(dc == DC - 1)),
                                            reads=[W_b, hT_b], writes=[b_mm], inc=(dc == DC - 1))
                                  fw.op(act, lambda: nc.scalar.copy(out=v_t[:, jb, :], in_=t_mm[:, :]), reads=[b_mm], writes=[v_b])
                          for g in range(4):
                              w = WINS[g]
                              for tc in range(NCH):
                                  t_mm, b_mm = mm.next()
                                  for dc in range(DC):
                                      fw.op(pe, lambda: nc.tensor.matmul(t_mm[:, :], lhsT=W[:, dc, g * 128:(g + 1) * 128],
                                                                         rhs=hT[:, dc, tc * 512:(tc + 1) * 512],
                                                                         start=(dc == 0), stop=(dc == DC - 1)),
                                            reads=[W_b, hT_b], writes=[b_mm], inc=(dc == DC - 1))
                                  fw.op(act, lambda: nc.scalar.copy(out=u_t[:, 16 + tc * 512:16 + (tc + 1) * 512], in_=t_mm[:, :]),
                                        reads=[b_mm], writes=[u_b])
                              nv = (SB + 3) // 4
                              emit_v(range(g * nv, min(SB, (g + 1) * nv)))
                              cur_t, cur_b = u_t, u_b
                              nxt = [(sa_t, sa_b), (sb2_t, sb2_b)]
                              sh = 1
                              ni = 0
                              while sh < w:
                                  o_t, o_b = nxt[ni % 2]
                                  fw.op(pool, lambda: nc.gpsimd.tensor_tensor(out=o_t[:, 16:16 + S], in0=cur_t[:, 16:16 + S],
                                                                              in1=cur_t[:, 16 - sh:16 - sh + S], op=ALU.add),
                                        reads=[cur_b], writes=[o_b])
                                  cur_t, cur_b = o_t, o_b
                                  ni += 1
                                  sh *= 2
                              fw.op(dve, lambda: nc.vector.scalar_tensor_tensor(out=pl_t[:, :], in0=cur_t[:, 16:16 + S],
                                                                                scalar=1.0 / w, in1=u_t[:, 16:16 + S],
                                                                                op0=ALU.mult, op1=ALU.subtract),
                                    reads=[cur_b, u_b], writes=[pl_b])
                              fw.op(dve, lambda: nc.vector.tensor_tensor(out=f16[:, :], in0=cur_t[:, 16:32],
                                                                         in1=cf[:, 544 + g * 16:544 + (g + 1) * 16], op=ALU.mult),
                                    reads=[cur_b, cf_b], writes=[f16_b])
                              fw.op(dve, lambda: nc.vector.tensor_tensor(out=pl_t[:, 0:16], in0=f16[:, :], in1=u_t[:, 16:32],
                                                                         op=ALU.subtract),
                                    reads=[f16_b, u_b, pl_b], writes=[pl_b])
                              for tc in range(NCH):
                                  t_mm, b_mm = mm.next()
                                  fw.op(pe, lambda: nc.tensor.matmul(t_mm[:, :], lhsT=wpl[:, g, :], rhs=pl_t[:, tc * 512:(tc + 1) * 512],
                                                                     start=True, stop=True),
                                        reads=[wpl_b, pl_b], writes=[b_mm])
                                  fw.op(act, lambda: nc.scalar.activation(out=ycatT[:, g, tc * 512:(tc + 1) * 512], in_=t_mm[:, :],
                                                                          func=AF.Copy, scale=psc[:, g:g + 1]),
                                        reads=[b_mm, psc_b], writes=[ycat_b])
                          _chk("P3")
                          fw.dma(pool, lambda: nc.gpsimd.dma_start(out=W[:, :, :], in_=win_l[:, :, 512:1536]),
                                 reads=[win_b], writes=[W_b])
                          _chk("P5")
                          for c in range(4):
                              for tc in range(NCH):
                                  sl = slice(tc * 512, (tc + 1) * 512)
                                  if c == 1:
                                      _chk("P6")
                                  t_mm, b_mm = mm.next()
                                  for dc in range(DC):
                                      fw.op(pe, lambda: nc.tensor.matmul(t_mm[:, :], lhsT=W[:, dc, c * 128:(c + 1) * 128],
                                                                         rhs=hT[:, dc, sl], start=(dc == 0), stop=(dc == DC - 1)),
                                            reads=[W_b, hT_b], writes=[b_mm], inc=(dc == DC - 1))
                                  fw.op(act, lambda: nc.scalar.mul(out=qk[:, 0, c, sl], in_=t_mm[:, :], mul=0.125),
                                        reads=[b_mm], writes=[q_b])
                                  t_mm, b_mm = mm.next()
                                  for dc in range(DC):
                                      fw.op(pe, lambda: nc.tensor.matmul(t_mm[:, :], lhsT=W[:, dc, 512 + c * 128:512 + (c + 1) * 128],
                                                                         rhs=hT[:, dc, sl], start=(dc == 0), stop=(dc == DC - 1)),
                                            reads=[W_b, hT_b], writes=[b_mm], inc=(dc == DC - 1))
                                  fw.op(act, lambda: nc.scalar.copy(out=qk[:, 1, c, sl], in_=t_mm[:, :]), reads=[b_mm], writes=[k_b])
                          fw.barrier()
                      ph = ExitStack()
                      if cfg.stop_after == ("P", l):
                          continue
                      with ph:
                          fw.dma(pool, lambda: nc.gpsimd.dma_start(out=wo[:], in_=w_out[l].rearrange("(c p) n -> p c n", p=128)),
                                 reads=[win_b], writes=[wo_b])
                          Zr = Rot(fw, ph, "Zr", [128, 512], F32, 2, psum=True)
                          Pr = Rot(fw, ph, "Pr", [128, 512], F32, 2, psum=True)
                          Qr = Rot(fw, ph, "Qr", [128, 512], F32, 2, psum=True)
                          Or = Rot(fw, ph, "Or", [128, 512], F32, 2, psum=True)
                          er = Rot(fw, ph, "er", [128, 512], F32, 6)
                          spr = Rot(fw, ph, "spr", [128, 512], BF16, 3)
                          ar = Rot(fw, ph, "ar", [128, 512], F32, 4)
                          Ar = Rot(fw, ph, "Ar", [128, 512], BF16, 3)
                          Cr = Rot(fw, ph, "Cr", [128, 512], F32, 2)
                          iters = []
                          for c in range(4):
                              for qc in range(NCH):
                                  O_t, O_b = Or.next()
                                  for hh in range(2):
                                      C_t, C_b = Cr.next()
                                      hs = slice(hh * 64, (hh + 1) * 64)
                                      h_col = (2 * c + hh) * 64
                                      first = True
                                      for m in range(4 * qc + 3, -1, -1):
                                          diag = m >= 4 * qc
                                          c0 = (m - 4 * qc) * 128 if diag else 0
                                          N = 512 - c0
                                          ms = slice(m * 128, (m + 1) * 128)
                                          qs = slice(qc * 512 + c0, (qc + 1) * 512)

                                          def mk(c=c, qc=qc, hh=hh, m=m, diag=diag, c0=c0, N=N, ms=ms, qs=qs, hs=hs,
                                                 h_col=h_col, O_t=O_t, O_b=O_b, C_t=C_t, C_b=C_b, first=first):
                                              st = {}

                                              def s0():
                                                  n_att_it[0] += 1
                                                  if pending and n_att_it[0] % cast_every == 0:
                                                      pending.pop(0)()
                                                  if first:
                                                      fw.op(pool, lambda: nc.gpsimd.memset(C_t[:, :], 0.0), writes=[C_b])
                                                      if hh == 0:
                                                          fw.op(dve, lambda: nc.vector.memset(O_t[:, :], 0.0), writes=[O_b])
                                                  st["Z"] = Zr.next()
                                                  Z_t, Z_b = st["Z"]
                                                  fw.op(pe, lambda: nc.tensor.matmul(Z_t[:, 0:N], lhsT=qk[hs, 1, c, ms], rhs=qk[hs, 0, c, qs],
                                                                                     start=True, stop=True),
                                                        reads=[q_b, k_b], writes=[Z_b])

                                              def s1():
                                                  Z_t, Z_b = st["Z"]
                                                  st["e"] = er.next()
                                                  e_t, e_b = st["e"]
                                                  fw.op(act, lambda: nc.scalar.activation(out=e_t[:, 0:N], in_=Z_t[:, 0:N], func=AF.Exp),
                                                        reads=[Z_b], writes=[e_b])
                                                  if diag:
                                                      fw.op(pool, lambda: nc.gpsimd.tensor_tensor(out=e_t[:, 0:128], in0=e_t[:, 0:128],
                                                                                                  in1=maskf, op=ALU.mult),
                                                            reads=[e_b, cf_b], writes=[e_b])

                                              def s2():
                                                  e_t, e_b = st["e"]
                                                  st["sp"] = spr.next()
                                                  s_t, s_b = st["sp"]
                                                  fw.op(act, lambda: nc.scalar.activation(out=s_t[:, 0:N], in_=e_t[:, 0:N], func=AF.Ln,
                                                                                          bias=onec),
                                                        reads=[e_b], writes=[s_b])

                                              def s3():
                                                  s_t, s_b = st["sp"]
                                                  st["P"] = Pr.next()
                                                  P_t, P_b = st["P"]
                                                  fw.op(pe, lambda: nc.tensor.matmul(P_t[:, 0:N], lhsT=triU, rhs=s_t[:, 0:N], start=True, stop=True),
                                                        reads=[s_b, cb_b], writes=[P_b])
                                                  st["Q"] = Qr.next()
                                                  Q_t, Q_b = st["Q"]
                                                  fw.op(pe, lambda: nc.tensor.matmul(Q_t[:, 0:N], lhsT=ones, rhs=s_t[:, 0:N], start=True, stop=True),
                                                        reads=[s_b, cb_b], writes=[Q_b])

                                              def s4():
                                                  P_t, P_b = st["P"]
                                                  Q_t, Q_b = st["Q"]
                                                  st["a"] = ar.next()
                                                  a_t, a_b = st["a"]
                                                  fw.op(dve, lambda: nc.vector.tensor_tensor(out=a_t[:, 0:N], in0=P_t[:, 0:N], in1=C_t[:, c0:512],
                                                                                             op=ALU.add),
                                                        reads=[P_b, C_b], writes=[a_b])
                                                  if m > 0:
                                                      fw.op(dve, lambda: nc.vector.tensor_tensor(out=C_t[:, c0:512], in0=Q_t[:, 0:N],
                                                                                                 in1=C_t[:, c0:512], op=ALU.add),
                                                            reads=[Q_b, C_b], writes=[C_b])

                                              def s5():
                                                  a_t, a_b = st["a"]
                                                  e_t, e_b = st["e"]
                                                  st["A"] = Ar.next()
                                                  A_t, A_b = st["A"]
                                                  fw.op(act, lambda: nc.scalar.activation(out=a_t[:, 0:N], in_=a_t[:, 0:N], func=AF.Exp, scale=-1.0),
                                                        reads=[a_b], writes=[a_b])
                                                  fw.op(pool, lambda: nc.gpsimd.tensor_tensor(out=A_t[:, 0:N], in0=a_t[:, 0:N], in1=e_t[:, 0:N],
                                                                                              op=ALU.mult),
                                                        reads=[a_b, e_b], writes=[A_b])

                                              def s6():
                                                  A_t, A_b = st["A"]
                                                  lhs = v_t[:, m, h_col:h_col + 64]
                                                  fw.op(pe, lambda: nc.tensor.matmul(O_t[hs, c0:512], lhsT=lhs, rhs=A_t[:, 0:N],
                                                                                     start=False, stop=(m == 0), skip_group_check=True),
                                                        reads=[A_b, v_b], writes=[O_b])
                                                  if m == 0 and hh == 1:
                                                      fw.op(act, lambda: nc.scalar.copy(out=ycatT[:, 4 + c, qc * 512:(qc + 1) * 512], in_=O_t[:, :]),
                                                            reads=[O_b], writes=[ycat_b])
                                              return [s0, s1, s2, s3, s4, s5, s6]
                                          iters.append(mk())
                                          first = False
                          pipeline(iters)
                          fw.barrier()
                      ph = ExitStack()
                      if cfg.stop_after == ("T", l):
                          with ExitStack() as dd:
                              yf = sb_t(dd, "yf", [128, 8 * S], F32); yf_b = fw.buf("yf")
                              fw.op(dve, lambda: nc.vector.tensor_copy(out=yf[:, :], in_=ycatT[:].rearrange("p a s -> p (a s)")),
                                    reads=[ycat_b], writes=[yf_b])
                              fw.dma(sp, lambda: nc.sync.dma_start(out=out.rearrange("(p a) d -> p (a d)", p=128), in_=yf[:, :]),
                                     reads=[yf_b], writes=[out_b[0]])
                              fw.barrier()
                          cfg.dbg_done = True
                          continue
                      with ph:
                          gB = sb_t(ph, "gB", [128, D], F32); bB = sb_t(ph, "bB", [128, D], F32); gb_b = fw.buf("gb")
                          wrf = sb_t(ph, "wrf", [128, DC, 36], F32); wrf_b = fw.buf("wrf")
                          wrhl = sb_t(ph, "wrhl", [128, DC, 72], BF16)
                          wrh = wrhl[:, :, 0:36]; wrl = wrhl[:, :, 36:72]
                          wr_b = fw.buf("wr")
                          brB = sb_t(ph, "brB", [128, 36], F32); br_b = fw.buf("br")
                          mixr = Rot(fw, ph, "mixr", [128, D], F32, 2, psum=True)
                          tp = Rot(fw, ph, "tpO", [128, DC, 128], BF16, 2, psum=True)
                          lgr = Rot(fw, ph, "lgr", [128, 512], F32, 2, psum=True)
                          hin = Rot(fw, ph, "hin", [128, D], F32, 3)
                          zr = Rot(fw, ph, "zr", [128, D], F32, 2)
                          znr = Rot(fw, ph, "znr", [128, D], F32, 2)
                          h1r = Rot(fw, ph, "h1r", [128, D], F32, 3)
                          hbr = Rot(fw, ph, "hbr", [128, D], BF16, 4)
                          lor = Rot(fw, ph, "lor", [128, D], BF16, 4)
                          hiT = Rot(fw, ph, "hiT", [128, DC, 128], BF16, 3)
                          loT = Rot(fw, ph, "loT", [128, DC, 128], BF16, 3)
                          str_ = Rot(fw, ph, "str", [128, 16], F32, 3)
                          fw.dma(sp, lambda: nc.sync.dma_start(out=gB[:], in_=ln1_g[l].partition_broadcast(128)),
                                 reads=[win_b], writes=[gb_b])
                          fw.dma(sp, lambda: nc.sync.dma_start(out=bB[:], in_=ln1_b[l].partition_broadcast(128)),
                                 reads=[win_b], cwrites=[gb_b])
                          with nc.allow_non_contiguous_dma(reason="tiny router weights"):
                              fw.dma(sp, lambda: nc.sync.dma_start(out=wrf[:, :, 0:NG], in_=w_rg[l].rearrange("(c p) n -> p c n", p=128)),
                                     reads=[win_b], writes=[wrf_b])
                              fw.dma(sp, lambda: nc.sync.dma_start(out=wrf[:, :, NG:36], in_=w_re[l].rearrange("(c p) n -> p c n", p=128)),
                                     reads=[win_b], cwrites=[wrf_b])
                          fw.dma(sp, lambda: nc.sync.dma_start(out=brB[:, 0:NG], in_=b_rg[l].partition_broadcast(128)),
                                 reads=[win_b], writes=[br_b])
                          fw.dma(sp, lambda: nc.sync.dma_start(out=brB[:, NG:36], in_=b_re[l].partition_broadcast(128)),
                                 reads=[win_b], cwrites=[br_b])
                          fw.op(pool, lambda: nc.gpsimd.tensor_copy(out=wrh, in_=wrf[:]), reads=[wrf_b], writes=[wr_b])
                          fw.op(dve, lambda: nc.vector.tensor_tensor(out=wrl, in0=wrf[:], in1=wrh, op=ALU.subtract),
                                reads=[wrf_b, wr_b], writes=[wr_b])
                          def mkO(jb):
                              j = b * SB + jb
                              ts = slice(jb * 128, (jb + 1) * 128)
                              st = {}

                              def sA(half):
                                  if half == 0:
                                      st["mix"] = mixr.next()
                                  mix_t, mix_b = st["mix"]
                                  for sl in range(8):
                                      fw.op(pe, lambda: nc.tensor.matmul(mix_t[:, half * 512:(half + 1) * 512], lhsT=ycatT[:, sl, ts],
                                                                         rhs=wo[:, sl, half * 512:(half + 1) * 512],
                                                                         start=(sl == 0), stop=(sl == 7)),
                                            reads=[ycat_b, wo_b], cwrites=[mix_b], inc=(sl == 7))
                                  if half == 1:
                                      st["in"] = hin.next()
                                      t_in, b_in = st["in"]
                                      fw.dma(sp, lambda: nc.sync.dma_start(out=t_in[:], in_=h_src[j * 128:(j + 1) * 128, :]),
                                             reads=[hsrc_b[j]], writes=[b_in])

                              def sB():
                                  mix_t, mix_b = st["mix"]
                                  t_in, b_in = st["in"]
                                  z_t, z_b = zr.next()
                                  fw.op(dve, lambda: nc.vector.scalar_tensor_tensor(out=z_t[:], in0=t_in[:], scalar=ALPHA, in1=mix_t[:],
                                                                                    op0=ALU.mult, op1=ALU.add),
                                        reads=[b_in, mix_b], writes=[z_b])
                                  h1_t, h1_b = layer_norm(z_t, z_b, gB, bB, gb_b, str_, znr, h1r)
                                  fw.dma(sp, lambda: nc.sync.dma_start(out=h1d[j * 128:(j + 1) * 128, :], in_=h1_t[:]),
                                         reads=[h1_b], writes=[h1d_b[j]])
                                  st["hb"] = hbr.next()
                                  hb_t, hb_b = st["hb"]
                                  fw.op(act, lambda: nc.scalar.copy(out=hb_t[:], in_=h1_t[:]), reads=[h1_b], writes=[hb_b])
                                  fw.dma(sp, lambda: nc.sync.dma_start(out=h1bd[j * 128:(j + 1) * 128, :], in_=hb_t[:]),
                                         reads=[hb_b], writes=[h1bd_b[j]])
                                  st["lo"] = lor.next()
                                  lo_t, lo_b = st["lo"]
                                  fw.op(dve, lambda: nc.vector.tensor_tensor(out=lo_t[:], in0=h1_t[:], in1=hb_t[:], op=ALU.subtract),
                                        reads=[h1_b, hb_b], writes=[lo_b])

                              def sThi():
                                  hb_t, hb_b = st["hb"]
                                  st["hiT"] = hiT.next()
                                  hi_T, hi_b = st["hiT"]
                                  transpose_block(hb_t, hb_b, tp, hi_T[:], hi_b, act)

                              def sTlo():
                                  lo_t, lo_b = st["lo"]
                                  st["loT"] = loT.next()
                                  lo_T, lo_Tb = st["loT"]
                                  transpose_block(lo_t, lo_b, tp, lo_T[:], lo_Tb, dve)

                              def sRa():
                                  hi_T, hi_b = st["hiT"]
                                  st["lg"] = lgr.next()
                                  lg_t, lg_b = st["lg"]
                                  for dc in range(DC):
                                      fw.op(pe, lambda: nc.tensor.matmul(lg_t[:, 0:72], lhsT=hi_T[:, dc, :], rhs=wrhl[:, dc, :],
                                                                         start=(dc == 0), stop=False, skip_group_check=True),
                                            reads=[hi_b, wr_b], writes=[lg_b], inc=(dc == DC - 1))

                              def sRb():
                                  lo_T, lo_Tb = st["loT"]
                                  lg_t, lg_b = st["lg"]
                                  for dc in range(DC):
                                      fw.op(pe, lambda: nc.tensor.matmul(lg_t[:, 0:36], lhsT=lo_T[:, dc, :], rhs=wrhl[:, dc, 0:36],
                                                                         start=False, stop=(dc == DC - 1), skip_group_check=True),
                                            reads=[lo_Tb, wr_b], writes=[lg_b], inc=(dc == DC - 1))
                                  fw.op(dve, lambda: nc.vector.tensor_tensor(out=L_t[:, j, :], in0=lg_t[:, 0:36], in1=brB[:], op=ALU.add),
                                        reads=[lg_b, br_b], writes=[L_b])
                                  fw.op(dve, lambda: nc.vector.tensor_tensor(out=L_t[:, j, :], in0=lg_t[:, 36:72], in1=L_t[:, j, :], op=ALU.add),
                                        reads=[lg_b, L_b], writes=[L_b])
                              return dict(A=sA, B=sB, Thi=sThi, Tlo=sTlo, Ra=sRa, Rb=sRb)
                          blks = [mkO(jb) for jb in range(SB)]

                          def at(i):
                              return blks[i] if 0 <= i < SB else None
                          for step in range(SB + 5):
                              bA, bB_, bT, bR = at(step), at(step - 1), at(step - 3), at(step - 4)
                              if bT: bT["Thi"]()
                              if bR: bR["Ra"]()
                              if bA: bA["A"](0)
                              if bT: bT["Tlo"]()
                              if bR: bR["Rb"]()
                              if bA: bA["A"](1)
                              if bB_: bB_["B"]()
                          fw.barrier()
              if cfg.stop_after in (("O", l), ("P", l), ("T", l), ("Z", l)):
                  break
              ph = ExitStack()
              with ph:
                  def rt(name, shape, dt=F32):
                      return sb_t(ph, name, shape, dt)
                  r_b = fw.buf("rtmp")
                  gmax = rt("gmax", [128, NB]); gm = rt("gm", [128, NB, NG]); gd = rt("gd", [128, NB, NG])
                  gsum = rt("gsum", [128, NB]); gp = rt("gp", [128, NB]); pen = rt("pen", [128, NB, NG])
                  elm = rt("elm", [128, NB, NE]); m1 = rt("m1", [128, NB, NE]); m2 = rt("m2", [128, NB, NE])
                  elm2 = rt("elm2", [128, NB, NE]); top1 = rt("top1", [128, NB]); top2 = rt("top2", [128, NB])
                  dl = rt("dl", [128, NB]); indb = rt("indb", [128, NB * NE], BF16)
                  pos = rt("pos", [128, NB, NE]); tot = rt("tot", [128, NB, NE]); off = rt("off", [128, NB, NE])
                  ov = rt("ov", [128, NB, NE]); tmp = rt("tmp", [128, NB, NE]); d1f = rt("d1f", [128, NB]); d2f = rt("d2f", [128, NB])
                  Wp = ps_t(ph, "Wp", [128, NB * NE], F32); Tp = ps_t(ph, "Tp", [128, NB * NE], F32)
                  wp_b = fw.buf("Wp")

                  def V(fn, extra_r=(), extra_w=()):
                      fw.op(dve, fn, reads=[r_b, L_b] + list(extra_r), writes=[r_b] + list(extra_w))

                  def bc2(ap2, n):
                      return ap2.unsqueeze(2).broadcast_to([128, NB, n])
                  gl = L_t[:, :, 0:NG]
                  el = L_t[:, :, NG:36]
                  V(lambda: nc.vector.tensor_reduce(out=gmax[:, :], in_=gl, axis=AX.X, op=ALU.max))
                  V(lambda: nc.vector.tensor_tensor(out=gm[:], in0=gl, in1=bc2(gmax[:, :], NG), op=ALU.is_equal))
                  V(lambda: nc.vector.tensor_tensor(out=gd[:], in0=gl, in1=bc2(gmax[:, :], NG), op=ALU.subtract))
                  fw.op(act, lambda: nc.scalar.activation(out=gd[:], in_=gd[:], func=AF.Exp), reads=[r_b], writes=[r_b])
                  V(lambda: nc.vector.tensor_reduce(out=gsum[:, :], in_=gd[:], axis=AX.X, op=ALU.add))
                  V(lambda: nc.vector.reciprocal(out=gp[:, :], in_=gsum[:, :]))
                  V(lambda: nc.vector.tensor_scalar(out=pen[:], in0=gm[:], scalar1=-1.0, scalar2=BIG, op0=ALU.add, op1=ALU.mult))
                  V(lambda: nc.vector.tensor_tensor(out=elm[:].rearrange("p j (g e) -> p j g e", g=NG),
                                                    in0=el.rearrange("p j (g e) -> p j g e", g=NG),
                                                    in1=pen[:].unsqueeze(3).broadcast_to([128, NB, NG, EPG]), op=ALU.add))
                  V(lambda: nc.vector.tensor_reduce(out=top1[:, :], in_=elm[:], axis=AX.X, op=ALU.max))
                  V(lambda: nc.vector.tensor_tensor(out=m1[:], in0=elm[:], in1=bc2(top1[:, :], NE), op=ALU.is_equal))
                  V(lambda: nc.vector.scalar_tensor_tensor(out=elm2[:], in0=m1[:], scalar=-BIG, in1=elm[:], op0=ALU.mult, op1=ALU.add))
                  V(lambda: nc.vector.tensor_reduce(out=top2[:, :], in_=elm2[:], axis=AX.X, op=ALU.max))
                  V(lambda: nc.vector.tensor_tensor(out=m2[:], in0=elm2[:], in1=bc2(top2[:, :], NE), op=ALU.is_equal))
                  V(lambda: nc.vector.tensor_tensor(out=dl[:, :], in0=top2[:, :], in1=top1[:, :], op=ALU.subtract))
                  fw.op(act, lambda: nc.scalar.activation(out=dl[:, :], in_=dl[:, :], func=AF.Exp), reads=[r_b], writes=[r_b])
                  V(lambda: nc.vector.tensor_scalar(out=dl[:, :], in0=dl[:, :], scalar1=1.0, scalar2=None, op0=ALU.add))
                  V(lambda: nc.vector.reciprocal(out=dl[:, :], in_=dl[:, :]))
                  V(lambda: nc.vector.tensor_tensor(out=g1[:, :], in0=gp[:, :], in1=dl[:, :], op=ALU.mult), extra_w=[rt_b])
                  V(lambda: nc.vector.tensor_tensor(out=g2[:, :], in0=gp[:, :], in1=g1[:, :], op=ALU.subtract), extra_w=[rt_b])
                  V(lambda: nc.vector.tensor_tensor(out=indb[:, :], in0=m1[:].rearrange("p j e -> p (j e)"),
                                                    in1=m2[:].rearrange("p j e -> p (j e)"), op=ALU.add))
                  ncol = NB * NE
                  for c0 in range(0, ncol, 512):
                      cn = min(512, ncol - c0)
                      fw.op(pe, lambda: nc.tensor.matmul(Wp[:, c0:c0 + cn], lhsT=triL, rhs=indb[:, c0:c0 + cn], start=True, stop=True),
                            reads=[r_b, cb_b], writes=[wp_b])
                      fw.op(pe, lambda: nc.tensor.matmul(Tp[:, c0:c0 + cn], lhsT=ones, rhs=indb[:, c0:c0 + cn], start=True, stop=True),
                            reads=[r_b, cb_b], writes=[wp_b])
                  V(lambda: nc.vector.tensor_copy(out=pos[:].rearrange("p j e -> p (j e)"), in_=Wp[:, :]), extra_r=[wp_b])
                  V(lambda: nc.vector.tensor_copy(out=tot[:].rearrange("p j e -> p (j e)"), in_=Tp[:, :]), extra_r=[wp_b])
                  V(lambda: nc.vector.memset(off[:, 0, :], 0.0))
                  for j in range(1, NB):
                      V(lambda: nc.vector.tensor_tensor(out=off[:, j, :], in0=off[:, j - 1, :], in1=tot[:, j - 1, :], op=ALU.add))
                  V(lambda: nc.vector.tensor_tensor(out=pos[:], in0=pos[:], in1=off[:], op=ALU.add))
                  V(lambda: nc.vector.tensor_scalar(out=ov[:], in0=pos[:], scalar1=float(C), scalar2=None, op0=ALU.is_ge))
                  V(lambda: nc.vector.tensor_tensor(out=pos[:], in0=pos[:], in1=eC.unsqueeze(1).broadcast_to([128, NB, NE]), op=ALU.add),
                    extra_r=[cf_b])
                  V(lambda: nc.vector.scalar_tensor_tensor(out=tmp[:], in0=pos[:], scalar=-float(P), in1=ov[:], op0=ALU.add, op1=ALU.mult))
                  V(lambda: nc.vector.tensor_tensor(out=pos[:], in0=pos[:], in1=tmp[:], op=ALU.subtract))
                  V(lambda: nc.vector.tensor_tensor(out=tmp[:], in0=m1[:], in1=pos[:], op=ALU.mult))
                  V(lambda: nc.vector.tensor_reduce(out=d1f[:, :], in_=tmp[:], axis=AX.X, op=ALU.add))
                  V(lambda: nc.vector.tensor_tensor(out=tmp[:], in0=m2[:], in1=pos[:], op=ALU.mult))
                  V(lambda: nc.vector.tensor_reduce(out=d2f[:, :], in_=tmp[:], axis=AX.X, op=ALU.add))
                  V(lambda: nc.vector.tensor_copy(out=d1i[:, :], in_=d1f[:, :]), extra_w=[rt_b])
                  V(lambda: nc.vector.tensor_copy(out=d2i[:, :], in_=d2f[:, :]), extra_w=[rt_b])
                  hbl = Rot(fw, ph, "hbl", [128, D], BF16, 3)
                  for j in range(NB):
                      t_h, b_h = hbl.next()
                      fw.dma(sp, lambda: nc.sync.dma_start(out=t_h[:], in_=h1bd[j * 128:(j + 1) * 128, :]),
                             reads=[h1bd_b[j]], writes=[b_h])
                      for di in (d1i, d2i):
                          fw.dma(pool, lambda: nc.gpsimd.indirect_dma_start(
                              out=xsd[:, :], out_offset=bass.IndirectOffsetOnAxis(ap=di[:, j:j + 1], axis=0),
                              in_=t_h[:, :], in_offset=None, bounds_check=reg_sc, oob_is_err=False),
                              reads=[b_h, rt_b], cwrites=[xsd_b])
                  fw.barrier()
              if cfg.stop_after == ("D", l):
                  break
              ph = ExitStack()
              with ph:
                  Wgr = Rot(fw, ph, "Wg", [128, DC, DE], BF16, 3)
                  Wur = Rot(fw, ph, "Wu", [128, DC, DE], BF16, 3)
                  Wdr = Rot(fw, ph, "Wd", [128, 4, D], BF16, 3)
                  xsr = Rot(fw, ph, "xs", [128, CB, D], BF16, 3)
                  xsTr = Rot(fw, ph, "xsT", [128, DC, C], BF16, 3)
                  hidr = Rot(fw, ph, "hid", [128, 4, C], BF16, 2)
                  sgr = Rot(fw, ph, "sg", [128, C], F32, 2)
                  ysr = Rot(fw, ph, "ys", [128, CB, D], F32, 2)
                  tpx = Rot(fw, ph, "tpx", [128, PSW], BF16, 2, psum=True)
                  Gr = Rot(fw, ph, "Gp", [128, 512], F32, 2, psum=True)
                  Ur = Rot(fw, ph, "Up", [128, 512], F32, 2, psum=True)
                  Yr = Rot(fw, ph, "Yp", [128, 512], F32, 2, psum=True)

                  while pending:
                      pending.pop(0)()
                  Wg_l, Wu_l, Wd_l = {}, {}, {}

                  def load_gu(e):
                      if e >= NE:
                          return
                      Wg, Wg_b = Wgr.next(); Wu, Wu_b = Wur.next()
                      fw.dma(sp, lambda: nc.sync.dma_start(out=Wg[:], in_=wbg[e].rearrange("(c p) f -> p c f", p=128)),
                             reads=[wb_b[e]], writes=[Wg_b])
                      fw.dma(sp, lambda: nc.sync.dma_start(out=Wu[:], in_=wbu[e].rearrange("(c p) f -> p c f", p=128)),
                             reads=[wb_b[e]], writes=[Wu_b])
                      Wg_l[e] = (Wg, Wg_b); Wu_l[e] = (Wu, Wu_b)

                  def load_d(e):
                      if e >= NE:
                          return
                      Wd, Wd_b = Wdr.next()
                      fw.dma(sp, lambda: nc.sync.dma_start(out=Wd[:], in_=wbd[e].rearrange("(c p) f -> p c f", p=128)),
                             reads=[wb_b[e]], writes=[Wd_b])
                      Wd_l[e] = (Wd, Wd_b)
                  xs_l, xT_l, hid_l = {}, {}, {}

                  def load_xs(e):
                      if e >= NE:
                          return
                      xs_t, xs_b = xsr.next()
                      fw.dma(sp, lambda: nc.sync.dma_start(out=xs_t[:], in_=xsd[e * C:(e + 1) * C, :].rearrange("(s p) d -> p s d", p=128)),
                             reads=[xsd_b], writes=[xs_b])
                      xs_l[e] = (xs_t, xs_b)
                      xT_l[e] = xsTr.next()

                  def tr_chunk(e, dc):
                      if e >= NE:
                          return
                      xs_t, xs_b = xs_l[e]
                      xT, xT_b = xT_l[e]
                      t_tp, b_tp = tpx.next()
                      for s_ in range(CB):
                          fw.op(pe, lambda: nc.tensor.transpose(out=t_tp[:, s_ * 128:(s_ + 1) * 128],
                                                                in_=xs_t[:, s_, dc * 128:(dc + 1) * 128], identity=ident),
                                reads=[xs_b, cb_b], writes=[b_tp], inc=(s_ == CB - 1))
                      if dc % 2 == 0:
                          fw.op(act, lambda: nc.scalar.copy(out=xT[:, dc, :], in_=t_tp[:, 0:C]), reads=[b_tp], cwrites=[xT_b])
                      else:
                          fw.op(dve, lambda: nc.vector.tensor_copy(out=xT[:, dc, :], in_=t_tp[:, 0:C]), reads=[b_tp], cwrites=[xT_b])

                  def gu(e):
                      Wg, Wg_b = Wg_l.pop(e); Wu, Wu_b = Wu_l.pop(e)
                      xT, xT_b = xT_l[e]
                      hid, hid_b = hidr.next()
                      hid_l[e] = (hid, hid_b)
                      for fc in range(4):
                          G_t, G_b = Gr.next(); U_t, U_b = Ur.next()
                          for dc in range(DC):
                              fw.op(pe, lambda: nc.tensor.matmul(G_t[:, 0:C], lhsT=Wg[:, dc, fc * 128:(fc + 1) * 128], rhs=xT[:, dc, :],
                                                                 start=(dc == 0), stop=(dc == DC - 1)),
                                    reads=[Wg_b, xT_b], writes=[G_b], inc=(dc == DC - 1))
                          for dc in range(DC):
                              fw.op(pe, lambda: nc.tensor.matmul(U_t[:, 0:C], lhsT=Wu[:, dc, fc * 128:(fc + 1) * 128], rhs=xT[:, dc, :],
                                                                 start=(dc == 0), stop=(dc == DC - 1)),
                                    reads=[Wu_b, xT_b], writes=[U_b], inc=(dc == DC - 1))
                          sg, sg_b = sgr.next()
                          fw.op(act, lambda: nc.scalar.activation(out=sg[:, :], in_=G_t[:, 0:C], func=AF.Silu), reads=[G_b], writes=[sg_b])
                          fw.op(dve, lambda: nc.vector.tensor_tensor(out=hid[:, fc, :], in0=sg[:, :], in1=U_t[:, 0:C], op=ALU.mult),
                                reads=[sg_b, U_b], cwrites=[hid_b])
                          tr_chunk(e + 1, 2 * fc)
                          tr_chunk(e + 1, 2 * fc + 1)

                  def dn(e):
                      hid, hid_b = hid_l.pop(e)
                      Wd, Wd_b = Wd_l.pop(e)
                      ys_t, ys_b = ysr.next()
                      k_ = 0
                      for s_ in range(CB):
                          for half in range(2):
                              Y_t, Y_b = Yr.next()
                              for fc in range(4):
                                  fw.op(pe, lambda: nc.tensor.matmul(Y_t[:, :], lhsT=hid[:, fc, s_ * 128:(s_ + 1) * 128],
                                                                     rhs=Wd[:, fc, half * 512:(half + 1) * 512],
                                                                     start=(fc == 0), stop=(fc == 3)),
                                        reads=[hid_b, Wd_b], writes=[Y_b], inc=(fc == 3))
                              if k_ % 2 == 0:
                                  fw.op(act, lambda: nc.scalar.copy(out=ys_t[:, s_, half * 512:(half + 1) * 512], in_=Y_t[:, :]),
                                        reads=[Y_b], cwrites=[ys_b])
                              else:
                                  fw.op(dve, lambda: nc.vector.tensor_copy(out=ys_t[:, s_, half * 512:(half + 1) * 512], in_=Y_t[:, :]),
                                        reads=[Y_b], cwrites=[ys_b])
                              k_ += 1
                      fw.dma(sp, lambda: nc.sync.dma_start(out=ysd[e * C:(e + 1) * C, :].rearrange("(s p) d -> p s d", p=128), in_=ys_t[:]),
                             reads=[ys_b], cwrites=[ysd_b])
                      xT_l.pop(e, None); xs_l.pop(e, None)

                  load_xs(0)
                  load_gu(0); load_d(0)
                  load_xs(1)
                  load_gu(1); load_d(1)
                  for dc in range(DC):
                      tr_chunk(0, dc)
                  for e in range(NE):
                      load_xs(e + 2)
                      load_gu(e + 2)
                      gu(e)
                      if e > 0:
                          dn(e - 1)
                      load_d(e + 2)
                  dn(NE - 1)
                  fw.barrier()
              ph = ExitStack()
              with ph:
                  gB = sb_t(ph, "gB2", [128, D], F32); bB = sb_t(ph, "bB2", [128, D], F32); gb_b = fw.buf("gb2")
                  y1r = Rot(fw, ph, "y1r", [128, D], F32, 4)
                  y2r = Rot(fw, ph, "y2r", [128, D], F32, 4)
                  hin = Rot(fw, ph, "hin2", [128, D], F32, 4)
                  zr = Rot(fw, ph, "zr2", [128, D], F32, 4)
                  znr = Rot(fw, ph, "znr2", [128, D], F32, 3)
                  h2r = Rot(fw, ph, "h2r", [128, D], F32, 3)
                  str_ = Rot(fw, ph, "str2", [128, 16], F32, 4)
                  fw.dma(sp, lambda: nc.sync.dma_start(out=gB[:], in_=ln2_g[l].partition_broadcast(128)), reads=[win_b], writes=[gb_b])
                  fw.dma(sp, lambda: nc.sync.dma_start(out=bB[:], in_=ln2_b[l].partition_broadcast(128)), reads=[win_b], cwrites=[gb_b])
                  def mkC(j):
                      st = {}

                      def sA():
                          st["y1"] = y1r.next(); st["y2"] = y2r.next()
                          for ((y_, yb_), di) in ((st["y1"], d1i), (st["y2"], d2i)):
                              fw.dma(pool, lambda: nc.gpsimd.indirect_dma_start(
                                  out=y_[:, :], out_offset=None, in_=ysd[:, :],
                                  in_offset=bass.IndirectOffsetOnAxis(ap=di[:, j:j + 1], axis=0),
                                  bounds_check=reg_ga, oob_is_err=False),
                                  reads=[ysd_b, rt_b], writes=[yb_])
                          st["in"] = hin.next()
                          t_in, b_in = st["in"]
                          fw.dma(sp, lambda: nc.sync.dma_start(out=t_in[:], in_=h1d[j * 128:(j + 1) * 128, :]), reads=[h1d_b[j]], writes=[b_in])

                      def sN():
                          pass

                      def sB():
                          y1, y1_b = st["y1"]; y2, y2_b = st["y2"]
                          t_in, b_in = st["in"]
                          z_t, z_b = zr.next()
                          fw.op(act, lambda: nc.scalar.mul(out=z_t[:], in_=t_in[:], mul=ALPHA), reads=[b_in], writes=[z_b])
                          fw.op(dve, lambda: nc.vector.scalar_tensor_tensor(out=z_t[:], in0=y1[:], scalar=g1[:, j:j + 1], in1=z_t[:],
                                                                            op0=ALU.mult, op1=ALU.add),
                                reads=[y1_b, rt_b, z_b], writes=[z_b])
                          fw.op(dve, lambda: nc.vector.scalar_tensor_tensor(out=z_t[:], in0=y2[:], scalar=g2[:, j:j + 1], in1=z_t[:],
                                                                            op0=ALU.mult, op1=ALU.add),
                                reads=[y2_b, rt_b, z_b], writes=[z_b])
                          st["z"] = (z_t, z_b)
                          st["st"] = ln_stats(z_t, z_b, str_)

                      def sB2():
                          z_t, z_b = st["z"]
                          st_t, st_b = st["st"]
                          h2_t, h2_b = ln_apply(z_t, z_b, st_t, st_b, gB, bB, gb_b, znr, h2r, badd_eng=dve)
                          if last:
                              fw.dma(sp, lambda: nc.sync.dma_start(out=out[j * 128:(j + 1) * 128, :], in_=h2_t[:]), reads=[h2_b], writes=[out_b[j]])
                          else:
                              fw.dma(sp, lambda: nc.sync.dma_start(out=hA[j * 128:(j + 1) * 128, :], in_=h2_t[:]), reads=[h2_b], writes=[hA_b[j]])
                      return [sA, sN, sN, sB, sB2]
                  pipeline([mkC(j) for j in range(NB)])
                  fw.barrier()
        except _Stop:
            fw.barrier()
        fw.dead = False
        if cfg.stop_after is not None and not getattr(cfg, "dbg_done", False):
            with ExitStack() as ds:
                tmpd = Rot(fw, ds, "tmpd", [128, D], F32, 2)
                for j in range(NB):
                    t_, b_ = tmpd.next()
                    fw.dma(sp, lambda: nc.sync.dma_start(out=t_[:], in_=h1d[j * 128:(j + 1) * 128, :]), reads=[h1d_b[j]], writes=[b_])
                    fw.dma(sp, lambda: nc.sync.dma_start(out=out[j * 128:(j + 1) * 128, :], in_=t_[:]), reads=[b_], writes=[out_b[j]])
            fw.barrier()
    print("instructions:", fw.n_inst, "epochs:", fw.epoch)
    return nc


INPUT_NAMES = ["x", "w_in", "w_pool", "pool_scale", "w_out", "ln1_g", "ln1_b", "w_router_group", "b_router_group",
               "w_router_expert", "b_router_expert", "w_gate", "w_up", "w_down", "ln2_g", "ln2_b"]
RENAME = {"w_router_group": "w_rg", "b_router_group": "b_rg", "w_router_expert": "w_re", "b_router_expert": "b_re"}


def run(cfg, inputs, n_cores=8, trace=False):
    nc = build(cfg)
    cst = make_consts(cfg)
    x = np.ascontiguousarray(inputs["x"], dtype=np.float32)
    B = x.shape[0]
    assert B == n_cores * cfg.NSEQ
    shared = {}
    for k in INPUT_NAMES[1:]:
        a = np.ascontiguousarray(inputs[k], dtype=np.float32)
        if k == "b_router_expert":
            a = a.reshape(a.shape[0], NE)
        shared[RENAME.get(k, k)] = a
    shared["cst"] = cst
    in_maps = []
    for c in range(n_cores):
        m = dict(shared)
        m["x"] = x[c * cfg.NSEQ:(c + 1) * cfg.NSEQ].reshape(cfg.T, D)
        in_maps.append(m)
    res = run_bass_kernel_spmd(nc, in_maps, core_ids=list(range(n_cores)), trace=trace)
    outs = [r["out"].reshape(cfg.NSEQ, cfg.S, D) for r in res.results]
    return np.concatenate(outs, axis=0), res


def kernel(**inputs):
    cfg = Cfg()
    out, _ = run(cfg, inputs, n_cores=8)
    return np.ascontiguousarray(out, dtype=np.float32)
```

```python
import numpy as np
import ml_dtypes
import concourse.bass as bass
import concourse.mybir as mybir
from concourse.bass_utils import run_bass_kernel_spmd

F32 = mybir.dt.float32
BF16 = mybir.dt.bfloat16
I32 = mybir.dt.int32
AF = mybir.ActivationFunctionType
ALU = mybir.AluOpType
AX = mybir.AxisListType

D = 1024
DC = 8
NH = 8
HD = 64
PW = 512
NE = 32
NG = 4
EPG = 8
DE = 512
WINS = (2, 4, 8, 16)
LN_EPS = 1e-5
ALPHA = (2.0 * 4) ** 0.25
BIG = 1.0e30


class Buf:
    __slots__ = ("name", "w", "r")

    def __init__(self, name):
        self.name = name
        self.w = {}
        self.r = {}


class Eng:
    def __init__(self, fw, name, h, compute=True):
        self.fw = fw
        self.name = name
        self.h = h
        self.compute = compute
        self.seen = {}
        self.count = 0
        self.sem = None
        self.key = None
        self.pool = []
        self.pool_i = 0


class FW:
    def __init__(self, nc, n_dma_sems=10):
        self.nc = nc
        self.epoch = 0
        self.bufs = []
        self.pe = Eng(self, "pe", nc.tensor)
        self.act = Eng(self, "act", nc.scalar)
        self.dve = Eng(self, "dve", nc.vector)
        self.pool = Eng(self, "pool", nc.gpsimd)
        self.sp = Eng(self, "sp", nc.sync, compute=False)
        self.engs = [self.pe, self.act, self.dve, self.pool, self.sp]
        for e in self.engs:
            if e.compute:
                self._new_sem(e)
        for e in (self.sp, self.pool, self.act):
            for i in range(n_dma_sems):
                s = nc.alloc_semaphore(f"dq_{e.name}_{i}")
                e.pool.append([f"dq_{e.name}_{i}", s, 0])
        self.n_inst = 0
        self.dead = False
        _FW[0] = self

    def _new_sem(self, e):
        e.sem = self.nc.alloc_semaphore(f"s_{e.name}_{self.epoch}")
        e.key = f"{e.name}@{self.epoch}"
        e.count = 0

    def buf(self, name):
        b = Buf(name)
        self.bufs.append(b)
        return b

    def _wait(self, e, key, sem, val):
        if e.seen.get(key, 0) >= val:
            return
        e.h.wait_ge(sem, val)
        e.seen[key] = val

    def _deps(self, e, reads, writes, dma, cwrites=()):
        for b in reads:
            for k, (s, v) in b.w.items():
                self._wait(e, k, s, v)
        for b in cwrites:
            for k, (s, v) in b.r.items():
                if dma or k != e.key:
                    self._wait(e, k, s, v)
        for b in writes:
            for k, (s, v) in b.w.items():
                if dma or k != e.key:
                    self._wait(e, k, s, v)
            for k, (s, v) in b.r.items():
                if dma or k != e.key:
                    self._wait(e, k, s, v)

    def _mark(self, key, sem, val, reads, writes, cwrites=()):
        for b in reads:
            b.r[key] = (sem, val)
        for b in cwrites:
            b.w[key] = (sem, val)
        for b in writes:
            b.w = {key: (sem, val)}
            b.r = {}

    def op(self, e, fn, reads=(), writes=(), inc=True, cwrites=()):
        if self.dead:
            return None
        self._deps(e, reads, writes, False, cwrites)
        ins = fn()
        self.n_inst += 1
        if inc:
            e.count += 1
            ins.then_inc(e.sem, 1)
            val = e.count
        else:
            val = e.count + 1
        self._mark(e.key, e.sem, val, reads, writes, cwrites)
        return ins

    def dma(self, e, fn, reads=(), writes=(), cwrites=()):
        if self.dead:
            return None
        slot = e.pool[e.pool_i]
        e.pool_i = (e.pool_i + 1) % len(e.pool)
        key, sem, val = slot
        if val:
            self._wait(e, key, sem, val)
        self._deps(e, reads, writes, True, cwrites)
        ins = fn()
        self.n_inst += 1
        val += 16
        slot[2] = val
        ins.then_inc(sem, 16)
        self._mark(key, sem, val, reads, writes, cwrites)
        return (key, sem, val)

    def barrier(self):
        if self.dead:
            return
        comp = [e for e in self.engs if e.compute]
        for e in self.engs:
            for x in comp:
                if x.count:
                    self._wait(e, x.key, x.sem, x.count)
            for q in (self.sp, self.pool, self.act):
                for key, sem, val in q.pool:
                    if val:
                        self._wait(e, key, sem, val)
        for b in self.bufs:
            b.w = {}
            b.r = {}
        if max(e.count for e in comp) > 12000:
            self.epoch += 1
            for e in comp:
                self._new_sem(e)


class Rot:
    def __init__(self, fw, es, name, shape, dtype, n, psum=False):
        self.items = []
        for i in range(n):
            _UID[0] += 1
            if psum:
                t = es.enter_context(fw.nc.psum_tensor(f"{name}{i}_{_UID[0]}", shape, dtype))
            else:
                t = es.enter_context(fw.nc.sbuf_tensor(f"{name}{i}_{_UID[0]}", shape, dtype))
            self.items.append((t, fw.buf(f"{name}{i}")))
        self.i = 0

    def next(self):
        it = self.items[self.i]
        self.i = (self.i + 1) % len(self.items)
        return it


class _Stop(Exception):
    pass


_FW = [None]
_UID = [0]


def _chk(tag):
    import os
    if os.environ.get("MK_STOP") == tag and not _FW[0].dead:
        _FW[0].barrier()
        _FW[0].dead = True


class Cfg:
    def __init__(self, S=2048, NSEQ=2, L=4, C=384, stop_after=None):
        self.S, self.NSEQ, self.L, self.C = S, NSEQ, L, C
        self.T = S * NSEQ
        self.NB = self.T // 128
        self.SB = S // 128
        self.NCH = S // 512
        self.P = NE * C
        self.CB = C // 128
        self.stop_after = stop_after


def make_consts(cfg):
    c = np.zeros((128, 640), np.float32)
    c[:, 608] = LN_EPS
    c[:, 609] = 1.0
    r = np.arange(128)
    c[:, 0:128] = np.eye(128)
    c[:, 128:256] = (r[:, None] >= r[None, :])
    c[:, 256:384] = 1.0
    c[:, 384:512] = (r[:, None] < r[None, :])
    c[:, 512:544] = (np.arange(NE) * cfg.C)[None, :]
    for g, w in enumerate(WINS):
        c[:, 544 + g * 16: 544 + (g + 1) * 16] = 1.0 / np.minimum(np.arange(16) + 1.0, float(w))[None, :]
    return c


def build(cfg):
    from contextlib import ExitStack
    nc = bass.Bass("TRN2", target_bir_lowering=False)
    fw = FW(nc)
    pe, act, dve, pool, sp = fw.pe, fw.act, fw.dve, fw.pool, fw.sp
    S, NSEQ, L, C, T, NB, SB, NCH, P, CB = (cfg.S, cfg.NSEQ, cfg.L, cfg.C, cfg.T, cfg.NB, cfg.SB,
                                            cfg.NCH, cfg.P, cfg.CB)
    PSW = 1024

    def din(name, shape):
        return nc.dram_tensor(name, list(shape), F32, kind="ExternalInput").ap()

    x = din("x", [T, D])
    w_in = din("w_in", [L, D, 2048])
    w_pool = din("w_pool", [L, 4, 128, 128])
    pool_scale = din("pool_scale", [L, 512])
    w_out = din("w_out", [L, D, D])
    ln1_g = din("ln1_g", [L, D]); ln1_b = din("ln1_b", [L, D])
    w_rg = din("w_rg", [L, D, NG]); b_rg = din("b_rg", [L, NG])
    w_re = din("w_re", [L, D, NE]); b_re = din("b_re", [L, NE])
    w_gate = din("w_gate", [L, NE, D, DE]); w_up = din("w_up", [L, NE, D, DE])
    w_down = din("w_down", [L, NE, DE, D])
    ln2_g = din("ln2_g", [L, D]); ln2_b = din("ln2_b", [L, D])
    cst = din("cst", [128, 640])
    out = nc.dram_tensor("out", [T, D], F32, kind="ExternalOutput").ap()

    def dscr(name, shape, dt):
        return nc.dram_tensor(name, list(shape), dt, kind="Internal").ap()

    hA = dscr("hA", [T, D], F32)
    h1d = dscr("h1d", [T, D], F32)
    h1bd = dscr("h1bd", [T, D], BF16)
    xsd = dscr("xsd", [P + 128, D], BF16)
    ysd = dscr("ysd", [P + 128, D], F32)
    wbg = dscr("wbg", [NE, D, DE], BF16)
    wbu = dscr("wbu", [NE, D, DE], BF16)
    wbd = dscr("wbd", [NE, DE, D], BF16)
    dbg = None
    if cfg.stop_after is not None:
        dbg = True

    hA_b = [fw.buf(f"hA{j}") for j in range(NB)]
    h1d_b = [fw.buf(f"h1d{j}") for j in range(NB)]
    h1bd_b = [fw.buf(f"h1bd{j}") for j in range(NB)]
    xsd_b = fw.buf("xsd")
    ysd_b = fw.buf("ysd")
    out_b = [fw.buf(f"out{j}") for j in range(NB)]
    win_b = fw.buf("weights")
    wb_b = [fw.buf(f"wb{e}") for e in range(NE)]
    pending = []

    es = ExitStack()
    with es:
        def sb_t(stack, name, shape, dt):
            _UID[0] += 1
            return stack.enter_context(nc.sbuf_tensor(f"{name}_{_UID[0]}", list(shape), dt))

        def ps_t(stack, name, shape, dt):
            _UID[0] += 1
            return stack.enter_context(nc.psum_tensor(f"{name}_{_UID[0]}", list(shape), dt))

        reg_sc = nc.gpsimd.to_reg(P - 1)
        reg_ga = nc.gpsimd.to_reg(P + 127)
        cf = sb_t(es, "cf", [128, 640], F32); cf_b = fw.buf("cf")
        cb = sb_t(es, "cb", [128, 512], BF16); cb_b = fw.buf("cb")
        fw.dma(sp, lambda: nc.sync.dma_start(out=cf[:], in_=cst[:, :]), reads=[win_b], writes=[cf_b])
        fw.op(dve, lambda: nc.vector.tensor_copy(out=cb[:], in_=cf[:, 0:512]), reads=[cf_b], writes=[cb_b])
        ident = cb[:, 0:128]
        triU = cb[:, 128:256]
        ones = cb[:, 256:384]
        triL = cb[:, 384:512]
        maskf = cf[:, 384:512]
        maskb = cb[:, 384:512]
        eC = cf[:, 512:544]
        epsc = cf[:, 608:609]
        onec = cf[:, 609:610]
        L_t = sb_t(es, "L_t", [128, NB, 36], F32); L_b = fw.buf("L")
        d1i = sb_t(es, "d1i", [128, NB], I32); d2i = sb_t(es, "d2i", [128, NB], I32)
        g1 = sb_t(es, "g1", [128, NB], F32); g2 = sb_t(es, "g2", [128, NB], F32)
        rt_b = fw.buf("route")
        with ExitStack() as zs:
            zf = sb_t(zs, "zf", [128, D], F32); zf_b = fw.buf("zf")
            zb = sb_t(zs, "zb", [128, D], BF16); zb_b = fw.buf("zb")
            fw.op(pool, lambda: nc.gpsimd.memset(zf[:], 0.0), writes=[zf_b])
            fw.op(pool, lambda: nc.gpsimd.memset(zb[:], 0.0), writes=[zb_b])
            fw.dma(sp, lambda: nc.sync.dma_start(out=ysd[P:P + 128, :], in_=zf[:]), reads=[zf_b], cwrites=[ysd_b])
            nrow = (P + 128) // 128
            r0 = 0
            import os
            if os.environ.get("MK_SKIP_ZERO"):
                r0 = nrow
            while r0 < nrow:
                rn = min(16, nrow - r0)
                dst = xsd[r0 * 128:(r0 + rn) * 128, :].rearrange("(r p) d -> p r d", p=128)
                srcz = zb[:, :].unsqueeze(1).broadcast_to([128, rn, D])
                fw.dma(sp, lambda: nc.sync.dma_start(out=dst, in_=srcz), reads=[zb_b], cwrites=[xsd_b])
                r0 += rn
            fw.barrier()

        def transpose_block(src_t, src_b, tp_rot, dst_ap, dst_b, copy_eng):
            t_tp, b_tp = tp_rot.next()
            for dc in range(DC):
                fw.op(pe, lambda: nc.tensor.transpose(out=t_tp[:, dc, :], in_=src_t[:, dc * 128:(dc + 1) * 128],
                                                      identity=ident),
                      reads=[src_b, cb_b], writes=[b_tp], inc=(dc == DC - 1))
            if copy_eng is act:
                fw.op(act, lambda: nc.scalar.copy(out=dst_ap, in_=t_tp[:]), reads=[b_tp], writes=[dst_b])
            else:
                fw.op(dve, lambda: nc.vector.tensor_copy(out=dst_ap, in_=t_tp[:]), reads=[b_tp], writes=[dst_b])

        def layer_norm(z_t, z_b, gB, bB, gb_b, st_rot, zn_rot, h_rot):
            st_t, st_b = st_rot.next()
            for i in range(2):
                fw.op(dve, lambda: nc.vector.bn_stats(out=st_t[:, i * 6:(i + 1) * 6], in_=z_t[:, i * 512:(i + 1) * 512]),
                      reads=[z_b], writes=[st_b])
            fw.op(dve, lambda: nc.vector.bn_aggr(out=st_t[:, 12:14], in_=st_t[:, 0:12]), reads=[st_b], writes=[st_b])
            fw.op(act, lambda: nc.scalar.activation(out=st_t[:, 14:15], in_=st_t[:, 13:14], func=AF.Ln, bias=epsc[:, 0:1]),
                  reads=[st_b, cf_b], writes=[st_b])
            fw.op(act, lambda: nc.scalar.activation(out=st_t[:, 14:15], in_=st_t[:, 14:15], func=AF.Exp, scale=-0.5),
                  reads=[st_b], writes=[st_b])
            fw.op(dve, lambda: nc.vector.tensor_scalar(out=st_t[:, 15:16], in0=st_t[:, 12:13], scalar1=st_t[:, 14:15],
                                                       scalar2=-1.0, op0=ALU.mult, op1=ALU.mult),
                  reads=[st_b], writes=[st_b])
            zn_t, zn_b = zn_rot.next()
            fw.op(act, lambda: nc.scalar.activation(out=zn_t[:], in_=z_t[:], func=AF.Identity,
                                                    bias=st_t[:, 15:16], scale=st_t[:, 14:15]),
                  reads=[z_b, st_b], writes=[zn_b])
            h_t, h_b = h_rot.next()
            fw.op(dve, lambda: nc.vector.tensor_tensor(out=h_t[:], in0=zn_t[:], in1=gB[:], op=ALU.mult),
                  reads=[zn_b, gb_b], writes=[h_b])
            fw.op(pool, lambda: nc.gpsimd.tensor_tensor(out=h_t[:], in0=h_t[:], in1=bB[:], op=ALU.add),
                  reads=[h_b, gb_b], writes=[h_b])
            return h_t, h_b

        def pipeline(iters):
            n = len(iters)
            ns = max(len(i) for i in iters) if iters else 0
            for step in range(n + ns - 1):
                for s in range(ns - 1, -1, -1):
                    i = step - s
                    if 0 <= i < n and s < len(iters[i]):
                        iters[i][s]()

        try:
          for l in range(L):
              h_src = x if l == 0 else hA
              hsrc_b = [win_b] * NB if l == 0 else hA_b
              last = (l == L - 1)

              def mkcast(e, dst, srcw):
                  def f():
                      fw.dma(pool, lambda: nc.gpsimd.dma_start(out=dst[e], in_=srcw[l, e]), reads=[win_b], cwrites=[wb_b[e]])
                  return f
              for e in range(NE):
                  pending.append(mkcast(e, wbg, w_gate))
                  pending.append(mkcast(e, wbu, w_up))
                  pending.append(mkcast(e, wbd, w_down))
              n_att_it = [0]
              tot_it = NSEQ * 4 * 2 * sum(4 * qc + 4 for qc in range(NCH))
              cast_every = max(1, (tot_it * 9 // 10) // (3 * NE))
              for b in range(NSEQ):
                  if cfg.stop_after == ("Z", l):
                      continue
                  seq = ExitStack()
                  with seq:
                      ycatT = sb_t(seq, "ycatT", [128, 8, S], BF16); ycat_b = fw.buf("ycatT")
                      v_t = sb_t(seq, "v_t", [128, SB, 512], BF16); v_b = fw.buf("v")
                      qk = sb_t(seq, "qk", [128, 2, 4, S], BF16)
                      wo = sb_t(seq, "wo", [128, 8, D], BF16); wo_b = fw.buf("wo")
                      q_b = fw.buf("q"); k_b = fw.buf("k"); kn_b = fw.buf("kn")
                      ph = ExitStack()
                      with ph:
                          hT = sb_t(ph, "hT", [128, DC, S], BF16); hT_b = fw.buf("hT")
                          W = sb_t(ph, "W", [128, DC, 1024], BF16); Wa_b = fw.buf("Wa"); Wb_b = fw.buf("Wb")
                          wpl = sb_t(ph, "wpl", [128, 4, 128], BF16); wpl_b = fw.buf("wpl")
                          psc = sb_t(ph, "psc", [128, 4], F32); psc_b = fw.buf("psc")
                          xin = Rot(fw, ph, "xin", [128, D], F32, 2)
                          xbf = Rot(fw, ph, "xbf", [128, D], BF16, 2)
                          tp = Rot(fw, ph, "tpP", [128, DC, 128], BF16, 2, psum=True)
                          mm = Rot(fw, ph, "mmP", [128, 512], F32, 4, psum=True)
                          u_t = sb_t(ph, "u_t", [128, 16 + S], F32); u_b = fw.buf("u")
                          sa_t = sb_t(ph, "sa_t", [128, 16 + S], F32); sa_b = fw.buf("sa")
                          sb2_t = sb_t(ph, "sb2_t", [128, 16 + S], F32); sb2_b = fw.buf("sb2")
                          pl_t = sb_t(ph, "pl_t", [128, S], BF16); pl_b = fw.buf("pl")
                          f16 = sb_t(ph, "f16", [128, 16], F32); f16_b = fw.buf("f16")
                          win_l = w_in[l].rearrange("(c p) n -> p c n", p=128)
                          fw.dma(pool, lambda: nc.gpsimd.dma_start(out=W[:, :, 0:512], in_=win_l[:, :, 0:512]),
                                 reads=[win_b], writes=[Wa_b])
                          fw.dma(pool, lambda: nc.gpsimd.dma_start(out=W[:, :, 512:1024], in_=win_l[:, :, 1536:2048]),
                                 reads=[win_b], writes=[Wb_b])
                          fw.dma(pool, lambda: nc.gpsimd.dma_start(out=wpl[:], in_=w_pool[l].rearrange("g c d -> c g d")),
                                 reads=[win_b], writes=[wpl_b])
                          with nc.allow_non_contiguous_dma(reason="tiny pool scale"):
                              fw.dma(sp, lambda: nc.sync.dma_start(out=psc[:], in_=pool_scale[l].rearrange("(g p) -> p g", p=128)),
                                     reads=[win_b], writes=[psc_b])
                          for t_, b_ in ((u_t, u_b), (sa_t, sa_b), (sb2_t, sb2_b)):
                              fw.op(pool, lambda: nc.gpsimd.memset(t_[:, 0:16], 0.0), writes=[b_])
                          _chk("P1")
                          for jb in range(SB):
                              j = b * SB + jb
                              t_in, b_in = xin.next()
                              fw.dma(sp, lambda: nc.sync.dma_start(out=t_in[:], in_=h_src[j * 128:(j + 1) * 128, :]),
                                     reads=[hsrc_b[j]], writes=[b_in])
                              t_bf, b_bf = xbf.next()
                              fw.op(dve, lambda: nc.vector.tensor_copy(out=t_bf[:], in_=t_in[:]), reads=[b_in], writes=[b_bf])
                              transpose_block(t_bf, b_bf, tp, hT[:, :, jb * 128:(jb + 1) * 128], hT_b, act)
                          _chk("P2")
                          def emit_v(jbs):
                              for jb in jbs:
                                  t_mm, b_mm = mm.next()
                                  for dc in range(DC):
                                      fw.op(pe, lambda: nc.tensor.matmul(t_mm[:, :], lhsT=hT[:, dc, jb * 128:(jb + 1) * 128],
                                                                         rhs=W[:, dc, 512:1024], start=(dc == 0), stop=(dc == DC - 1)),
                                            reads=[Wb_b, hT_b], writes=[b_mm], inc=(dc == DC - 1))
                                  fw.op(act, lambda: nc.scalar.copy(out=v_t[:, jb, :], in_=t_mm[:, :]), reads=[b_mm], writes=[v_b])
                          for g in range(4):
                              w = WINS[g]
                              for tc in range(NCH):
                                  t_mm, b_mm = mm.next()
                                  for dc in range(DC):
                                      fw.op(pe, lambda: nc.tensor.matmul(t_mm[:, :], lhsT=W[:, dc, g * 128:(g + 1) * 128],
                                                                         rhs=hT[:, dc, tc * 512:(tc + 1) * 512],
                                                                         start=(dc == 0), stop=(dc == DC - 1)),
                                            reads=[Wa_b, hT_b], writes=[b_mm], inc=(dc == DC - 1))
                                  fw.op(act, lambda: nc.scalar.copy(out=u_t[:, 16 + tc * 512:16 + (tc + 1) * 512], in_=t_mm[:, :]),
                                        reads=[b_mm], writes=[u_b])
                              if g == 3:
                                  fw.dma(pool, lambda: nc.gpsimd.dma_start(out=W[:, :, 0:512], in_=win_l[:, :, 512:1024]),
                                         reads=[win_b], writes=[Wa_b])
                              nv = (SB + 3) // 4
                              emit_v(range(g * nv, min(SB, (g + 1) * nv)))
                              if g == 3:
                                  fw.dma(pool, lambda: nc.gpsimd.dma_start(out=W[:, :, 512:1024], in_=win_l[:, :, 1024:1536]),
                                         reads=[win_b], writes=[Wb_b])
                              cur_t, cur_b = u_t, u_b
                              nxt = [(sa_t, sa_b), (sb2_t, sb2_b)]
                              sh = 1
                              ni = 0
                              while sh < w:
                                  o_t, o_b = nxt[ni % 2]
                                  fw.op(pool, lambda: nc.gpsimd.tensor_tensor(out=o_t[:, 16:16 + S], in0=cur_t[:, 16:16 + S],
                                                                              in1=cur_t[:, 16 - sh:16 - sh + S], op=ALU.add),
                                        reads=[cur_b], writes=[o_b])
                                  cur_t, cur_b = o_t, o_b
                                  ni += 1
                                  sh *= 2
                              fw.op(dve, lambda: nc.vector.scalar_tensor_tensor(out=pl_t[:, :], in0=cur_t[:, 16:16 + S],
                                                                                scalar=1.0 / w, in1=u_t[:, 16:16 + S],
                                                                                op0=ALU.mult, op1=ALU.subtract),
                                    reads=[cur_b, u_b], writes=[pl_b])
                              fw.op(dve, lambda: nc.vector.tensor_tensor(out=f16[:, :], in0=cur_t[:, 16:32],
                                                                         in1=cf[:, 544 + g * 16:544 + (g + 1) * 16], op=ALU.mult),
                                    reads=[cur_b, cf_b], writes=[f16_b])
                              fw.op(dve, lambda: nc.vector.tensor_tensor(out=pl_t[:, 0:16], in0=f16[:, :], in1=u_t[:, 16:32],
                                                                         op=ALU.subtract),
                                    reads=[f16_b, u_b, pl_b], writes=[pl_b])
                              for tc in range(NCH):
                                  t_mm, b_mm = mm.next()
                                  fw.op(pe, lambda: nc.tensor.matmul(t_mm[:, :], lhsT=wpl[:, g, :], rhs=pl_t[:, tc * 512:(tc + 1) * 512],
                                                                     start=True, stop=True),
                                        reads=[wpl_b, pl_b], writes=[b_mm])
                                  fw.op(act, lambda: nc.scalar.activation(out=ycatT[:, g, tc * 512:(tc + 1) * 512], in_=t_mm[:, :],
                                                                          func=AF.Copy, scale=psc[:, g:g + 1]),
                                        reads=[b_mm, psc_b], writes=[ycat_b])
                          _chk("P3")
                          _chk("P5")
                          for c in range(4):
                              for tc in range(NCH):
                                  sl = slice(tc * 512, (tc + 1) * 512)
                                  t_mm, b_mm = mm.next()
                                  for dc in range(DC):
                                      fw.op(pe, lambda: nc.tensor.matmul(t_mm[:, :], lhsT=W[:, dc, c * 128:(c + 1) * 128],
                                                                         rhs=hT[:, dc, sl], start=(dc == 0), stop=(dc == DC - 1)),
                                            reads=[Wa_b, hT_b], writes=[b_mm], inc=(dc == DC - 1))
                                  fw.op(act, lambda: nc.scalar.mul(out=qk[:, 0, c, sl], in_=t_mm[:, :], mul=0.125),
                                        reads=[b_mm], writes=[q_b])
                              for tc in range(NCH):
                                  sl = slice(tc * 512, (tc + 1) * 512)
                                  t_mm, b_mm = mm.next()
                                  for dc in range(DC):
                                      fw.op(pe, lambda: nc.tensor.matmul(t_mm[:, :], lhsT=W[:, dc, 512 + c * 128:512 + (c + 1) * 128],
                                                                         rhs=hT[:, dc, sl], start=(dc == 0), stop=(dc == DC - 1)),
                                            reads=[Wb_b, hT_b], writes=[b_mm], inc=(dc == DC - 1))
                                  fw.op(act, lambda: nc.scalar.copy(out=qk[:, 1, c, sl], in_=t_mm[:, :]), reads=[b_mm], writes=[k_b])
                          fw.barrier()
                      ph = ExitStack()
                      if cfg.stop_after == ("P", l):
                          continue
                      with ph:
                          fw.dma(pool, lambda: nc.gpsimd.dma_start(out=wo[:], in_=w_out[l].rearrange("(c p) n -> p c n", p=128)),
                                 reads=[win_b], writes=[wo_b])
                          Zr = Rot(fw, ph, "Zr", [128, 512], F32, 2, psum=True)
                          Pr = Rot(fw, ph, "Pr", [128, 512], F32, 2, psum=True)
                          Qr = Rot(fw, ph, "Qr", [128, 512], F32, 2, psum=True)
                          Or = Rot(fw, ph, "Or", [128, 512], F32, 2, psum=True)
                          er = Rot(fw, ph, "er", [128, 512], F32, 6)
                          spr = Rot(fw, ph, "spr", [128, 512], BF16, 3)
                          ar = Rot(fw, ph, "ar", [128, 512], F32, 4)
                          Ar = Rot(fw, ph, "Ar", [128, 512], BF16, 3)
                          Cr = Rot(fw, ph, "Cr", [128, 512], F32, 2)
                          iters = []
                          for c in range(4):
                              for qc in range(NCH):
                                  O_t, O_b = Or.next()
                                  for hh in range(2):
                                      C_t, C_b = Cr.next()
                                      hs = slice(hh * 64, (hh + 1) * 64)
                                      h_col = (2 * c + hh) * 64
                                      first = True
                                      for m in range(4 * qc + 3, -1, -1):
                                          diag = m >= 4 * qc
                                          c0 = (m - 4 * qc) * 128 if diag else 0
                                          N = 512 - c0
                                          ms = slice(m * 128, (m + 1) * 128)
                                          qs = slice(qc * 512 + c0, (qc + 1) * 512)

                                          def mk(c=c, qc=qc, hh=hh, m=m, diag=diag, c0=c0, N=N, ms=ms, qs=qs, hs=hs,
                                                 h_col=h_col, O_t=O_t, O_b=O_b, C_t=C_t, C_b=C_b, first=first):
                                              st = {}

                                              def s0():
                                                  n_att_it[0] += 1
                                                  if pending and n_att_it[0] % cast_every == 0:
                                                      pending.pop(0)()
                                                  if first:
                                                      fw.op(pool, lambda: nc.gpsimd.memset(C_t[:, :], 0.0), writes=[C_b])
                                                      if hh == 0:
                                                          fw.op(dve, lambda: nc.vector.memset(O_t[:, :], 0.0), writes=[O_b])
                                                  st["Z"] = Zr.next()
                                                  Z_t, Z_b = st["Z"]
                                                  fw.op(pe, lambda: nc.tensor.matmul(Z_t[:, 0:N], lhsT=qk[hs, 1, c, ms], rhs=qk[hs, 0, c, qs],
                                                                                     start=True, stop=True),
                                                        reads=[q_b, k_b], writes=[Z_b])

                                              def s1():
                                                  Z_t, Z_b = st["Z"]
                                                  st["e"] = er.next()
                                                  e_t, e_b = st["e"]
                                                  fw.op(act, lambda: nc.scalar.activation(out=e_t[:, 0:N], in_=Z_t[:, 0:N], func=AF.Exp),
                                                        reads=[Z_b], writes=[e_b])
                                                  if diag:
                                                      fw.op(pool, lambda: nc.gpsimd.tensor_tensor(out=e_t[:, 0:128], in0=e_t[:, 0:128],
                                                                                                  in1=maskf, op=ALU.mult),
                                                            reads=[e_b, cf_b], writes=[e_b])

                                              def s2():
                                                  e_t, e_b = st["e"]
                                                  st["sp"] = spr.next()
                                                  s_t, s_b = st["sp"]
                                                  fw.op(act, lambda: nc.scalar.activation(out=s_t[:, 0:N], in_=e_t[:, 0:N], func=AF.Ln,
                                                                                          bias=onec),
                                                        reads=[e_b], writes=[s_b])

                                              def s3():
                                                  s_t, s_b = st["sp"]
                                                  st["P"] = Pr.next()
                                                  P_t, P_b = st["P"]
                                                  fw.op(pe, lambda: nc.tensor.matmul(P_t[:, 0:N], lhsT=triU, rhs=s_t[:, 0:N], start=True, stop=True),
                                                        reads=[s_b, cb_b], writes=[P_b])
                                                  st["Q"] = Qr.next()
                                                  Q_t, Q_b = st["Q"]
                                                  fw.op(pe, lambda: nc.tensor.matmul(Q_t[:, 0:N], lhsT=ones, rhs=s_t[:, 0:N], start=True, stop=True),
                                                        reads=[s_b, cb_b], writes=[Q_b])

                                              def s4():
                                                  P_t, P_b = st["P"]
                                                  Q_t, Q_b = st["Q"]
                                                  st["a"] = ar.next()
                                                  a_t, a_b = st["a"]
                                                  fw.op(dve, lambda: nc.vector.tensor_tensor(out=a_t[:, 0:N], in0=P_t[:, 0:N], in1=C_t[:, c0:512],
                                                                                             op=ALU.add),
                                                        reads=[P_b, C_b], writes=[a_b])
                                                  if m > 0:
                                                      fw.op(dve, lambda: nc.vector.tensor_tensor(out=C_t[:, c0:512], in0=Q_t[:, 0:N],
                                                                                                 in1=C_t[:, c0:512], op=ALU.add),
                                                            reads=[Q_b, C_b], writes=[C_b])

                                              def s5():
                                                  a_t, a_b = st["a"]
                                                  e_t, e_b = st["e"]
                                                  st["A"] = Ar.next()
                                                  A_t, A_b = st["A"]
                                                  fw.op(act, lambda: nc.scalar.activation(out=a_t[:, 0:N], in_=a_t[:, 0:N], func=AF.Exp, scale=-1.0),
                                                        reads=[a_b], writes=[a_b])
                                                  fw.op(pool, lambda: nc.gpsimd.tensor_tensor(out=A_t[:, 0:N], in0=a_t[:, 0:N], in1=e_t[:, 0:N],
                                                                                              op=ALU.mult),
                                                        reads=[a_b, e_b], writes=[A_b])

                                              def s6():
                                                  A_t, A_b = st["A"]
                                                  lhs = v_t[:, m, h_col:h_col + 64]
                                                  fw.op(pe, lambda: nc.tensor.matmul(O_t[hs, c0:512], lhsT=lhs, rhs=A_t[:, 0:N],
                                                                                     start=False, stop=(m == 0), skip_group_check=True),
                                                        reads=[A_b, v_b], writes=[O_b])
                                                  if m == 0 and hh == 1:
                                                      fw.op(dve, lambda: nc.vector.tensor_copy(out=ycatT[:, 4 + c, qc * 512:(qc + 1) * 512], in_=O_t[:, :]),
                                                            reads=[O_b], writes=[ycat_b])
                                              return [s0, s1, s2, s3, s4, s5, s6]
                                          iters.append(mk())
                                          first = False
                          pipeline(iters)
                          fw.barrier()
                      ph = ExitStack()
                      if cfg.stop_after == ("T", l):
                          with ExitStack() as dd:
                              yf = sb_t(dd, "yf", [128, 8 * S], F32); yf_b = fw.buf("yf")
                              fw.op(dve, lambda: nc.vector.tensor_copy(out=yf[:, :], in_=ycatT[:].rearrange("p a s -> p (a s)")),
                                    reads=[ycat_b], writes=[yf_b])
                              fw.dma(sp, lambda: nc.sync.dma_start(out=out.rearrange("(p a) d -> p (a d)", p=128), in_=yf[:, :]),
                                     reads=[yf_b], writes=[out_b[0]])
                              fw.barrier()
                          cfg.dbg_done = True
                          continue
                      with ph:
                          gB = sb_t(ph, "gB", [128, D], F32); bB = sb_t(ph, "bB", [128, D], F32); gb_b = fw.buf("gb")
                          wrf = sb_t(ph, "wrf", [128, DC, 36], F32); wrf_b = fw.buf("wrf")
                          wrhl = sb_t(ph, "wrhl", [128, DC, 72], BF16)
                          wrh = wrhl[:, :, 0:36]; wrl = wrhl[:, :, 36:72]
                          wr_b = fw.buf("wr")
                          brB = sb_t(ph, "brB", [128, 36], F32); br_b = fw.buf("br")
                          mixr = Rot(fw, ph, "mixr", [128, D], F32, 2, psum=True)
                          tp = Rot(fw, ph, "tpO", [128, DC, 128], BF16, 2, psum=True)
                          lgr = Rot(fw, ph, "lgr", [128, 512], F32, 2, psum=True)
                          hin = Rot(fw, ph, "hin", [128, D], F32, 3)
                          zr = Rot(fw, ph, "zr", [128, D], F32, 2)
                          znr = Rot(fw, ph, "znr", [128, D], F32, 2)
                          h1r = Rot(fw, ph, "h1r", [128, D], F32, 3)
                          hbr = Rot(fw, ph, "hbr", [128, D], BF16, 4)
                          lor = Rot(fw, ph, "lor", [128, D], BF16, 4)
                          hiT = Rot(fw, ph, "hiT", [128, DC, 128], BF16, 3)
                          loT = Rot(fw, ph, "loT", [128, DC, 128], BF16, 3)
                          str_ = Rot(fw, ph, "str", [128, 16], F32, 3)
                          fw.dma(sp, lambda: nc.sync.dma_start(out=gB[:], in_=ln1_g[l].partition_broadcast(128)),
                                 reads=[win_b], writes=[gb_b])
                          fw.dma(sp, lambda: nc.sync.dma_start(out=bB[:], in_=ln1_b[l].partition_broadcast(128)),
                                 reads=[win_b], cwrites=[gb_b])
                          with nc.allow_non_contiguous_dma(reason="tiny router weights"):
                              fw.dma(sp, lambda: nc.sync.dma_start(out=wrf[:, :, 0:NG], in_=w_rg[l].rearrange("(c p) n -> p c n", p=128)),
                                     reads=[win_b], writes=[wrf_b])
                              fw.dma(sp, lambda: nc.sync.dma_start(out=wrf[:, :, NG:36], in_=w_re[l].rearrange("(c p) n -> p c n", p=128)),
                                     reads=[win_b], cwrites=[wrf_b])
                          fw.dma(sp, lambda: nc.sync.dma_start(out=brB[:, 0:NG], in_=b_rg[l].partition_broadcast(128)),
                                 reads=[win_b], writes=[br_b])
                          fw.dma(sp, lambda: nc.sync.dma_start(out=brB[:, NG:36], in_=b_re[l].partition_broadcast(128)),
                                 reads=[win_b], cwrites=[br_b])
                          fw.op(pool, lambda: nc.gpsimd.tensor_copy(out=wrh, in_=wrf[:]), reads=[wrf_b], writes=[wr_b])
                          fw.op(dve, lambda: nc.vector.tensor_tensor(out=wrl, in0=wrf[:], in1=wrh, op=ALU.subtract),
                                reads=[wrf_b, wr_b], writes=[wr_b])
                          def mkO(jb):
                              j = b * SB + jb
                              ts = slice(jb * 128, (jb + 1) * 128)
                              st = {}

                              def sA(half):
                                  if half == 0:
                                      st["mix"] = mixr.next()
                                  mix_t, mix_b = st["mix"]
                                  for sl in range(8):
                                      fw.op(pe, lambda: nc.tensor.matmul(mix_t[:, half * 512:(half + 1) * 512], lhsT=ycatT[:, sl, ts],
                                                                         rhs=wo[:, sl, half * 512:(half + 1) * 512],
                                                                         start=(sl == 0), stop=(sl == 7)),
                                            reads=[ycat_b, wo_b], cwrites=[mix_b], inc=(sl == 7))
                                  if half == 1:
                                      st["in"] = hin.next()
                                      t_in, b_in = st["in"]
                                      fw.dma(sp, lambda: nc.sync.dma_start(out=t_in[:], in_=h_src[j * 128:(j + 1) * 128, :]),
                                             reads=[hsrc_b[j]], writes=[b_in])

                              def sB():
                                  mix_t, mix_b = st["mix"]
                                  t_in, b_in = st["in"]
                                  z_t, z_b = zr.next()
                                  fw.op(dve, lambda: nc.vector.scalar_tensor_tensor(out=z_t[:], in0=t_in[:], scalar=ALPHA, in1=mix_t[:],
                                                                                    op0=ALU.mult, op1=ALU.add),
                                        reads=[b_in, mix_b], writes=[z_b])
                                  h1_t, h1_b = layer_norm(z_t, z_b, gB, bB, gb_b, str_, znr, h1r)
                                  fw.dma(sp, lambda: nc.sync.dma_start(out=h1d[j * 128:(j + 1) * 128, :], in_=h1_t[:]),
                                         reads=[h1_b], writes=[h1d_b[j]])
                                  st["hb"] = hbr.next()
                                  hb_t, hb_b = st["hb"]
                                  fw.op(act, lambda: nc.scalar.copy(out=hb_t[:], in_=h1_t[:]), reads=[h1_b], writes=[hb_b])
                                  fw.dma(sp, lambda: nc.sync.dma_start(out=h1bd[j * 128:(j + 1) * 128, :], in_=hb_t[:]),
                                         reads=[hb_b], writes=[h1bd_b[j]])
                                  st["lo"] = lor.next()
                                  lo_t, lo_b = st["lo"]
                                  fw.op(dve, lambda: nc.vector.tensor_tensor(out=lo_t[:], in0=h1_t[:], in1=hb_t[:], op=ALU.subtract),
                                        reads=[h1_b, hb_b], writes=[lo_b])

                              def sThi():
                                  hb_t, hb_b = st["hb"]
                                  st["hiT"] = hiT.next()
                                  hi_T, hi_b = st["hiT"]
                                  transpose_block(hb_t, hb_b, tp, hi_T[:], hi_b, act)

                              def sTlo():
                                  lo_t, lo_b = st["lo"]
                                  st["loT"] = loT.next()
                                  lo_T, lo_Tb = st["loT"]
                                  transpose_block(lo_t, lo_b, tp, lo_T[:], lo_Tb, dve)

                              def sRa():
                                  hi_T, hi_b = st["hiT"]
                                  st["lg"] = lgr.next()
                                  lg_t, lg_b = st["lg"]
                                  for dc in range(DC):
                                      fw.op(pe, lambda: nc.tensor.matmul(lg_t[:, 0:72], lhsT=hi_T[:, dc, :], rhs=wrhl[:, dc, :],
                                                                         start=(dc == 0), stop=False, skip_group_check=True),
                                            reads=[hi_b, wr_b], writes=[lg_b], inc=(dc == DC - 1))

                              def sRb():
                                  lo_T, lo_Tb = st["loT"]
                                  lg_t, lg_b = st["lg"]
                                  for dc in range(DC):
                                      fw.op(pe, lambda: nc.tensor.matmul(lg_t[:, 0:36], lhsT=lo_T[:, dc, :], rhs=wrhl[:, dc, 0:36],
                                                                         start=False, stop=(dc == DC - 1), skip_group_check=True),
                                            reads=[lo_Tb, wr_b], writes=[lg_b], inc=(dc == DC - 1))
                                  fw.op(dve, lambda: nc.vector.tensor_tensor(out=L_t[:, j, :], in0=lg_t[:, 0:36], in1=brB[:], op=ALU.add),
                                        reads=[lg_b, br_b], writes=[L_b])
                                  fw.op(dve, lambda: nc.vector.tensor_tensor(out=L_t[:, j, :], in0=lg_t[:, 36:72], in1=L_t[:, j, :], op=ALU.add),
                                        reads=[lg_b, L_b], writes=[L_b])
                              return dict(A=sA, B=sB, Thi=sThi, Tlo=sTlo, Ra=sRa, Rb=sRb)
                          blks = [mkO(jb) for jb in range(SB)]

                          def at(i):
                              return blks[i] if 0 <= i < SB else None
                          for step in range(SB + 5):
                              bA, bB_, bT, bR = at(step), at(step - 1), at(step - 3), at(step - 4)
                              if bT: bT["Thi"]()
                              if bR: bR["Ra"]()
                              if bA: bA["A"](0)
                              if bT: bT["Tlo"]()
                              if bR: bR["Rb"]()
                              if bA: bA["A"](1)
                              if bB_: bB_["B"]()
                          fw.barrier()
              if cfg.stop_after in (("O", l), ("P", l), ("T", l), ("Z", l)):
                  break
              ph = ExitStack()
              with ph:
                  def rt(name, shape, dt=F32):
                      return sb_t(ph, name, shape, dt)
                  r_b = fw.buf("rtmp")
                  gmax = rt("gmax", [128, NB]); gm = rt("gm", [128, NB, NG]); gd = rt("gd", [128, NB, NG])
                  gsum = rt("gsum", [128, NB]); gp = rt("gp", [128, NB]); pen = rt("pen", [128, NB, NG])
                  elm = rt("elm", [128, NB, NE]); m1 = rt("m1", [128, NB, NE]); m2 = rt("m2", [128, NB, NE])
                  elm2 = rt("elm2", [128, NB, NE]); top1 = rt("top1", [128, NB]); top2 = rt("top2", [128, NB])
                  dl = rt("dl", [128, NB]); indb = rt("indb", [128, NB * NE], BF16)
                  pos = rt("pos", [128, NB, NE]); tot = rt("tot", [128, NB, NE]); off = rt("off", [128, NB, NE])
                  ov = rt("ov", [128, NB, NE]); tmp = rt("tmp", [128, NB, NE]); d1f = rt("d1f", [128, NB]); d2f = rt("d2f", [128, NB])
                  Wp = ps_t(ph, "Wp", [128, NB * NE], F32); Tp = ps_t(ph, "Tp", [128, NB * NE], F32)
                  wp_b = fw.buf("Wp")

                  def V(fn, extra_r=(), extra_w=()):
                      fw.op(dve, fn, reads=[r_b, L_b] + list(extra_r), writes=[r_b] + list(extra_w))

                  def bc2(ap2, n):
                      return ap2.unsqueeze(2).broadcast_to([128, NB, n])
                  gl = L_t[:, :, 0:NG]
                  el = L_t[:, :, NG:36]
                  V(lambda: nc.vector.tensor_reduce(out=gmax[:, :], in_=gl, axis=AX.X, op=ALU.max))
                  V(lambda: nc.vector.tensor_tensor(out=gm[:], in0=gl, in1=bc2(gmax[:, :], NG), op=ALU.is_equal))
                  V(lambda: nc.vector.tensor_tensor(out=gd[:], in0=gl, in1=bc2(gmax[:, :], NG), op=ALU.subtract))
                  fw.op(act, lambda: nc.scalar.activation(out=gd[:], in_=gd[:], func=AF.Exp), reads=[r_b], writes=[r_b])
                  V(lambda: nc.vector.tensor_reduce(out=gsum[:, :], in_=gd[:], axis=AX.X, op=ALU.add))
                  V(lambda: nc.vector.reciprocal(out=gp[:, :], in_=gsum[:, :]))
                  V(lambda: nc.vector.tensor_scalar(out=pen[:], in0=gm[:], scalar1=-1.0, scalar2=BIG, op0=ALU.add, op1=ALU.mult))
                  V(lambda: nc.vector.tensor_tensor(out=elm[:].rearrange("p j (g e) -> p j g e", g=NG),
                                                    in0=el.rearrange("p j (g e) -> p j g e", g=NG),
                                                    in1=pen[:].unsqueeze(3).broadcast_to([128, NB, NG, EPG]), op=ALU.add))
                  V(lambda: nc.vector.tensor_reduce(out=top1[:, :], in_=elm[:], axis=AX.X, op=ALU.max))
                  V(lambda: nc.vector.tensor_tensor(out=m1[:], in0=elm[:], in1=bc2(top1[:, :], NE), op=ALU.is_equal))
                  V(lambda: nc.vector.scalar_tensor_tensor(out=elm2[:], in0=m1[:], scalar=-BIG, in1=elm[:], op0=ALU.mult, op1=ALU.add))
                  V(lambda: nc.vector.tensor_reduce(out=top2[:, :], in_=elm2[:], axis=AX.X, op=ALU.max))
                  V(lambda: nc.vector.tensor_tensor(out=m2[:], in0=elm2[:], in1=bc2(top2[:, :], NE), op=ALU.is_equal))
                  V(lambda: nc.vector.tensor_tensor(out=dl[:, :], in0=top2[:, :], in1=top1[:, :], op=ALU.subtract))
                  fw.op(act, lambda: nc.scalar.activation(out=dl[:, :], in_=dl[:, :], func=AF.Exp), reads=[r_b], writes=[r_b])
                  V(lambda: nc.vector.tensor_scalar(out=dl[:, :], in0=dl[:, :], scalar1=1.0, scalar2=None, op0=ALU.add))
                  V(lambda: nc.vector.reciprocal(out=dl[:, :], in_=dl[:, :]))
                  V(lambda: nc.vector.tensor_tensor(out=g1[:, :], in0=gp[:, :], in1=dl[:, :], op=ALU.mult), extra_w=[rt_b])
                  V(lambda: nc.vector.tensor_tensor(out=g2[:, :], in0=gp[:, :], in1=g1[:, :], op=ALU.subtract), extra_w=[rt_b])
                  V(lambda: nc.vector.tensor_tensor(out=indb[:, :], in0=m1[:].rearrange("p j e -> p (j e)"),
                                                    in1=m2[:].rearrange("p j e -> p (j e)"), op=ALU.add))
                  ncol = NB * NE
                  for c0 in range(0, ncol, 512):
                      cn = min(512, ncol - c0)
                      fw.op(pe, lambda: nc.tensor.matmul(Wp[:, c0:c0 + cn], lhsT=triL, rhs=indb[:, c0:c0 + cn], start=True, stop=True),
                            reads=[r_b, cb_b], writes=[wp_b])
                      fw.op(pe, lambda: nc.tensor.matmul(Tp[:, c0:c0 + cn], lhsT=ones, rhs=indb[:, c0:c0 + cn], start=True, stop=True),
                            reads=[r_b, cb_b], writes=[wp_b])
                  V(lambda: nc.vector.tensor_copy(out=pos[:].rearrange("p j e -> p (j e)"), in_=Wp[:, :]), extra_r=[wp_b])
                  V(lambda: nc.vector.tensor_copy(out=tot[:].rearrange("p j e -> p (j e)"), in_=Tp[:, :]), extra_r=[wp_b])
                  V(lambda: nc.vector.memset(off[:, 0, :], 0.0))
                  for j in range(1, NB):
                      V(lambda: nc.vector.tensor_tensor(out=off[:, j, :], in0=off[:, j - 1, :], in1=tot[:, j - 1, :], op=ALU.add))
                  V(lambda: nc.vector.tensor_tensor(out=pos[:], in0=pos[:], in1=off[:], op=ALU.add))
                  V(lambda: nc.vector.tensor_scalar(out=ov[:], in0=pos[:], scalar1=float(C), scalar2=None, op0=ALU.is_ge))
                  V(lambda: nc.vector.tensor_tensor(out=pos[:], in0=pos[:], in1=eC.unsqueeze(1).broadcast_to([128, NB, NE]), op=ALU.add),
                    extra_r=[cf_b])
                  V(lambda: nc.vector.scalar_tensor_tensor(out=tmp[:], in0=pos[:], scalar=-float(P), in1=ov[:], op0=ALU.add, op1=ALU.mult))
                  V(lambda: nc.vector.tensor_tensor(out=pos[:], in0=pos[:], in1=tmp[:], op=ALU.subtract))
                  V(lambda: nc.vector.tensor_tensor(out=tmp[:], in0=m1[:], in1=pos[:], op=ALU.mult))
                  V(lambda: nc.vector.tensor_reduce(out=d1f[:, :], in_=tmp[:], axis=AX.X, op=ALU.add))
                  V(lambda: nc.vector.tensor_tensor(out=tmp[:], in0=m2[:], in1=pos[:], op=ALU.mult))
                  V(lambda: nc.vector.tensor_reduce(out=d2f[:, :], in_=tmp[:], axis=AX.X, op=ALU.add))
                  V(lambda: nc.vector.tensor_copy(out=d1i[:, :], in_=d1f[:, :]), extra_w=[rt_b])
                  V(lambda: nc.vector.tensor_copy(out=d2i[:, :], in_=d2f[:, :]), extra_w=[rt_b])
                  hbl = Rot(fw, ph, "hbl", [128, D], BF16, 3)
                  for j in range(NB):
                      t_h, b_h = hbl.next()
                      fw.dma(sp, lambda: nc.sync.dma_start(out=t_h[:], in_=h1bd[j * 128:(j + 1) * 128, :]),
                             reads=[h1bd_b[j]], writes=[b_h])
                      for di in (d1i, d2i):
                          fw.dma(pool, lambda: nc.gpsimd.indirect_dma_start(
                              out=xsd[:, :], out_offset=bass.IndirectOffsetOnAxis(ap=di[:, j:j + 1], axis=0),
                              in_=t_h[:, :], in_offset=None, bounds_check=reg_sc, oob_is_err=False),
                              reads=[b_h, rt_b], cwrites=[xsd_b])
                  fw.barrier()
              if cfg.stop_after == ("D", l):
                  break
              ph = ExitStack()
              with ph:
                  Wgr = Rot(fw, ph, "Wg", [128, DC, DE], BF16, 3)
                  Wur = Rot(fw, ph, "Wu", [128, DC, DE], BF16, 3)
                  Wdr = Rot(fw, ph, "Wd", [128, 4, D], BF16, 3)
                  xsr = Rot(fw, ph, "xs", [128, CB, D], BF16, 3)
                  xsTr = Rot(fw, ph, "xsT", [128, DC, C], BF16, 3)
                  hidr = Rot(fw, ph, "hid", [128, 4, C], BF16, 2)
                  sgr = Rot(fw, ph, "sg", [128, C], F32, 2)
                  ysr = Rot(fw, ph, "ys", [128, CB, D], F32, 2)
                  tpx = Rot(fw, ph, "tpx", [128, PSW], BF16, 2, psum=True)
                  Gr = Rot(fw, ph, "Gp", [128, 512], F32, 2, psum=True)
                  Ur = Rot(fw, ph, "Up", [128, 512], F32, 2, psum=True)
                  Yr = Rot(fw, ph, "Yp", [128, 512], F32, 2, psum=True)

                  while pending:
                      pending.pop(0)()
                  Wg_l, Wu_l, Wd_l = {}, {}, {}

                  def load_gu(e):
                      if e >= NE:
                          return
                      Wg, Wg_b = Wgr.next(); Wu, Wu_b = Wur.next()
                      fw.dma(sp, lambda: nc.sync.dma_start(out=Wg[:], in_=wbg[e].rearrange("(c p) f -> p c f", p=128)),
                             reads=[wb_b[e]], writes=[Wg_b])
                      fw.dma(sp, lambda: nc.sync.dma_start(out=Wu[:], in_=wbu[e].rearrange("(c p) f -> p c f", p=128)),
                             reads=[wb_b[e]], writes=[Wu_b])
                      Wg_l[e] = (Wg, Wg_b); Wu_l[e] = (Wu, Wu_b)

                  def load_d(e):
                      if e >= NE:
                          return
                      Wd, Wd_b = Wdr.next()
                      fw.dma(sp, lambda: nc.sync.dma_start(out=Wd[:], in_=wbd[e].rearrange("(c p) f -> p c f", p=128)),
                             reads=[wb_b[e]], writes=[Wd_b])
                      Wd_l[e] = (Wd, Wd_b)
                  xs_l, xT_l, hid_l = {}, {}, {}

                  def load_xs(e):
                      if e >= NE:
                          return
                      xs_t, xs_b = xsr.next()
                      fw.dma(sp, lambda: nc.sync.dma_start(out=xs_t[:], in_=xsd[e * C:(e + 1) * C, :].rearrange("(s p) d -> p s d", p=128)),
                             reads=[xsd_b], writes=[xs_b])
                      xs_l[e] = (xs_t, xs_b)
                      xT_l[e] = xsTr.next()

                  def tr_chunk(e, dc):
                      if e >= NE:
                          return
                      xs_t, xs_b = xs_l[e]
                      xT, xT_b = xT_l[e]
                      t_tp, b_tp = tpx.next()
                      for s_ in range(CB):
                          fw.op(pe, lambda: nc.tensor.transpose(out=t_tp[:, s_ * 128:(s_ + 1) * 128],
                                                                in_=xs_t[:, s_, dc * 128:(dc + 1) * 128], identity=ident),
                                reads=[xs_b, cb_b], writes=[b_tp], inc=(s_ == CB - 1))
                      if dc % 2 == 0:
                          fw.op(act, lambda: nc.scalar.copy(out=xT[:, dc, :], in_=t_tp[:, 0:C]), reads=[b_tp], cwrites=[xT_b])
                      else:
                          fw.op(dve, lambda: nc.vector.tensor_copy(out=xT[:, dc, :], in_=t_tp[:, 0:C]), reads=[b_tp], cwrites=[xT_b])

                  def gu(e):
                      Wg, Wg_b = Wg_l.pop(e); Wu, Wu_b = Wu_l.pop(e)
                      xT, xT_b = xT_l[e]
                      hid, hid_b = hidr.next()
                      hid_l[e] = (hid, hid_b)
                      for fc in range(4):
                          G_t, G_b = Gr.next(); U_t, U_b = Ur.next()
                          for dc in range(DC):
                              fw.op(pe, lambda: nc.tensor.matmul(G_t[:, 0:C], lhsT=Wg[:, dc, fc * 128:(fc + 1) * 128], rhs=xT[:, dc, :],
                                                                 start=(dc == 0), stop=(dc == DC - 1)),
                                    reads=[Wg_b, xT_b], writes=[G_b], inc=(dc == DC - 1))
                          for dc in range(DC):
                              fw.op(pe, lambda: nc.tensor.matmul(U_t[:, 0:C], lhsT=Wu[:, dc, fc * 128:(fc + 1) * 128], rhs=xT[:, dc, :],
                                                                 start=(dc == 0), stop=(dc == DC - 1)),
                                    reads=[Wu_b, xT_b], writes=[U_b], inc=(dc == DC - 1))
                          sg, sg_b = sgr.next()
                          fw.op(act, lambda: nc.scalar.activation(out=sg[:, :], in_=G_t[:, 0:C], func=AF.Silu), reads=[G_b], writes=[sg_b])
                          fw.op(dve, lambda: nc.vector.tensor_tensor(out=hid[:, fc, :], in0=sg[:, :], in1=U_t[:, 0:C], op=ALU.mult),
                                reads=[sg_b, U_b], cwrites=[hid_b])
                          tr_chunk(e + 1, 2 * fc)
                          tr_chunk(e + 1, 2 * fc + 1)

                  def dn(e):
                      hid, hid_b = hid_l.pop(e)
                      Wd, Wd_b = Wd_l.pop(e)
                      ys_t, ys_b = ysr.next()
                      k_ = 0
                      for s_ in range(CB):
                          for half in range(2):
                              Y_t, Y_b = Yr.next()
                              for fc in range(4):
                                  fw.op(pe, lambda: nc.tensor.matmul(Y_t[:, :], lhsT=hid[:, fc, s_ * 128:(s_ + 1) * 128],
                                                                     rhs=Wd[:, fc, half * 512:(half + 1) * 512],
                                                                     start=(fc == 0), stop=(fc == 3)),
                                        reads=[hid_b, Wd_b], writes=[Y_b], inc=(fc == 3))
                              if k_ % 2 == 0:
                                  fw.op(act, lambda: nc.scalar.copy(out=ys_t[:, s_, half * 512:(half + 1) * 512], in_=Y_t[:, :]),
                                        reads=[Y_b], cwrites=[ys_b])
                              else:
                                  fw.op(dve, lambda: nc.vector.tensor_copy(out=ys_t[:, s_, half * 512:(half + 1) * 512], in_=Y_t[:, :]),
                                        reads=[Y_b], cwrites=[ys_b])
                              k_ += 1
                      fw.dma(sp, lambda: nc.sync.dma_start(out=ysd[e * C:(e + 1) * C, :].rearrange("(s p) d -> p s d", p=128), in_=ys_t[:]),
                             reads=[ys_b], cwrites=[ysd_b])
                      xT_l.pop(e, None); xs_l.pop(e, None)

                  load_xs(0)
                  load_gu(0); load_d(0)
                  load_xs(1)
                  load_gu(1); load_d(1)
                  for dc in range(DC):
                      tr_chunk(0, dc)
                  for e in range(NE):
                      load_xs(e + 2)
                      load_gu(e + 2)
                      gu(e)
                      if e > 0:
                          dn(e - 1)
                      load_d(e + 2)
                  dn(NE - 1)
                  fw.barrier()
              ph = ExitStack()
              with ph:
                  gB = sb_t(ph, "gB2", [128, D], F32); bB = sb_t(ph, "bB2", [128, D], F32); gb_b = fw.buf("gb2")
                  y1r = Rot(fw, ph, "y1r", [128, D], F32, 4)
                  y2r = Rot(fw, ph, "y2r", [128, D], F32, 4)
                  hin = Rot(fw, ph, "hin2", [128, D], F32, 4)
                  zr = Rot(fw, ph, "zr2", [128, D], F32, 3)
                  znr = Rot(fw, ph, "znr2", [128, D], F32, 3)
                  h2r = Rot(fw, ph, "h2r", [128, D], F32, 3)
                  str_ = Rot(fw, ph, "str2", [128, 16], F32, 4)
                  fw.dma(sp, lambda: nc.sync.dma_start(out=gB[:], in_=ln2_g[l].partition_broadcast(128)), reads=[win_b], writes=[gb_b])
                  fw.dma(sp, lambda: nc.sync.dma_start(out=bB[:], in_=ln2_b[l].partition_broadcast(128)), reads=[win_b], cwrites=[gb_b])
                  def mkC(j):
                      st = {}

                      def sA():
                          st["y1"] = y1r.next(); st["y2"] = y2r.next()
                          for ((y_, yb_), di) in ((st["y1"], d1i), (st["y2"], d2i)):
                              fw.dma(pool, lambda: nc.gpsimd.indirect_dma_start(
                                  out=y_[:, :], out_offset=None, in_=ysd[:, :],
                                  in_offset=bass.IndirectOffsetOnAxis(ap=di[:, j:j + 1], axis=0),
                                  bounds_check=reg_ga, oob_is_err=False),
                                  reads=[ysd_b, rt_b], writes=[yb_])
                          st["in"] = hin.next()
                          t_in, b_in = st["in"]
                          fw.dma(sp, lambda: nc.sync.dma_start(out=t_in[:], in_=h1d[j * 128:(j + 1) * 128, :]), reads=[h1d_b[j]], writes=[b_in])

                      def sN():
                          pass

                      def sB():
                          y1, y1_b = st["y1"]; y2, y2_b = st["y2"]
                          t_in, b_in = st["in"]
                          z_t, z_b = zr.next()
                          fw.op(act, lambda: nc.scalar.mul(out=z_t[:], in_=t_in[:], mul=ALPHA), reads=[b_in], writes=[z_b])
                          fw.op(dve, lambda: nc.vector.scalar_tensor_tensor(out=z_t[:], in0=y1[:], scalar=g1[:, j:j + 1], in1=z_t[:],
                                                                            op0=ALU.mult, op1=ALU.add),
                                reads=[y1_b, rt_b, z_b], writes=[z_b])
                          fw.op(dve, lambda: nc.vector.scalar_tensor_tensor(out=z_t[:], in0=y2[:], scalar=g2[:, j:j + 1], in1=z_t[:],
                                                                            op0=ALU.mult, op1=ALU.add),
                                reads=[y2_b, rt_b, z_b], writes=[z_b])
                          h2_t, h2_b = layer_norm(z_t, z_b, gB, bB, gb_b, str_, znr, h2r)
                          if last:
                              fw.dma(sp, lambda: nc.sync.dma_start(out=out[j * 128:(j + 1) * 128, :], in_=h2_t[:]), reads=[h2_b], writes=[out_b[j]])
                          else:
                              fw.dma(sp, lambda: nc.sync.dma_start(out=hA[j * 128:(j + 1) * 128, :], in_=h2_t[:]), reads=[h2_b], writes=[hA_b[j]])
                      return [sA, sN, sN, sB]
                  pipeline([mkC(j) for j in range(NB)])
                  fw.barrier()
        except _Stop:
            fw.barrier()
        fw.dead = False
        if cfg.stop_after is not None and not getattr(cfg, "dbg_done", False):
            with ExitStack() as ds:
                tmpd = Rot(fw, ds, "tmpd", [128, D], F32, 2)
                for j in range(NB):
                    t_, b_ = tmpd.next()
                    fw.dma(sp, lambda: nc.sync.dma_start(out=t_[:], in_=h1d[j * 128:(j + 1) * 128, :]), reads=[h1d_b[j]], writes=[b_])
                    fw.dma(sp, lambda: nc.sync.dma_start(out=out[j * 128:(j + 1) * 128, :], in_=t_[:]), reads=[b_], writes=[out_b[j]])
            fw.barrier()
    print("instructions:", fw.n_inst, "epochs:", fw.epoch)
    return nc


INPUT_NAMES = ["x", "w_in", "w_pool", "pool_scale", "w_out", "ln1_g", "ln1_b", "w_router_group", "b_router_group",
               "w_router_expert", "b_router_expert", "w_gate", "w_up", "w_down", "ln2_g", "ln2_b"]
RENAME = {"w_router_group": "w_rg", "b_router_group": "b_rg", "w_router_expert": "w_re", "b_router_expert": "b_re"}


def run(cfg, inputs, n_cores=8, trace=False):
    nc = build(cfg)
    cst = make_consts(cfg)
    x = np.ascontiguousarray(inputs["x"], dtype=np.float32)
    B = x.shape[0]
    assert B == n_cores * cfg.NSEQ
    shared = {}
    for k in INPUT_NAMES[1:]:
        a = np.ascontiguousarray(inputs[k], dtype=np.float32)
        if k == "b_router_expert":
            a = a.reshape(a.shape[0], NE)
        shared[RENAME.get(k, k)] = a
    shared["cst"] = cst
    in_maps = []
    for c in range(n_cores):
        m = dict(shared)
        m["x"] = x[c * cfg.NSEQ:(c + 1) * cfg.NSEQ].reshape(cfg.T, D)
        in_maps.append(m)
    res = run_bass_kernel_spmd(nc, in_maps, core_ids=list(range(n_cores)), trace=trace)
    outs = [r["out"].reshape(cfg.NSEQ, cfg.S, D) for r in res.results]
    return np.concatenate(outs, axis=0), res


def kernel(**inputs):
    cfg = Cfg()
    out, _ = run(cfg, inputs, n_cores=8)
    return np.ascontiguousarray(out, dtype=np.float32)
```
